# Optimizing a Trainium2 kernel written in Bass

```python
import jax, jax.numpy as jnp
from jax import lax
import numpy as np

D_MODEL = 1024
BATCH = 4
SEQ = 4096
DEPTH = 2
DEC_BATCH = 32
DEC_SEQ = 1
PAST_LEN = 8192
PAGE_SIZE = 128

N_EVEN = (DEPTH + 1) // 2
N_ODD = DEPTH // 2
MIX_WIDTH = D_MODEL
A_WIDTH = MIX_WIDTH // 2
B_WIDTH = MIX_WIDTH - A_WIDTH
POOL_WINDOWS = (2, 4, 8, 16)
POOL_GROUP = A_WIDTH // len(POOL_WINDOWS)
POOL_HIST = max(POOL_WINDOWS) - 1
CONV_B_WIDTH = 31
N_HEADS = 8
HEAD_DIM = 64
C_WIDTH = N_HEADS * HEAD_DIM
D_WIDTH = MIX_WIDTH - C_WIDTH
CONV_D_WIDTH = 3
Q_BLOCK = 128
D_FF = 2816
N_EXPERTS = 8
TOP_K = 2
D_FF_EXPERT = 3584
ALPHA = (2 * DEPTH) ** 0.25
BETA = (8 * DEPTH) ** -0.25
FORGET_BIAS = 3.0
LN_EPS = 1e-5
EVEN_IN = A_WIDTH + 2 * B_WIDTH
ODD_IN = 3 * C_WIDTH + N_HEADS + 3 * D_WIDTH

kernel_name = 'hybrid_pool_conformer_fox_shortconv_decode_step'


def layer_norm(x, g, b):
    xf = x.astype(jnp.float32)
    mu = jnp.mean(xf, axis=-1, keepdims=True)
    var = jnp.mean(jnp.square(xf - mu), axis=-1, keepdims=True)
    y = (xf - mu) * lax.rsqrt(var + LN_EPS) * g.astype(jnp.float32) + b.astype(jnp.float32)
    return y.astype(x.dtype)


def causal_dwconv(u_ext, w):
    return lax.conv_general_dilated(u_ext, w[:, None, :], window_strides=(1,), padding='VALID',
                                    dimension_numbers=('NWC', 'WIO', 'NWC'),
                                    feature_group_count=u_ext.shape[-1])


def multiscale_pool(a_ext, n_hist, first_pos, pool_w, pool_scale):
    T = a_ext.shape[1] - n_hist
    cs = jnp.cumsum(jnp.pad(a_ext.astype(jnp.float32), ((0, 0), (1, 0), (0, 0))), axis=1)
    hi = n_hist + jnp.arange(T) + 1
    pos = first_pos + jnp.arange(T)
    cur = a_ext[:, n_hist:].astype(jnp.float32)
    outs = []
    for g, w in enumerate(POOL_WINDOWS):
        sl = slice(g * POOL_GROUP, (g + 1) * POOL_GROUP)
        csg = cs[..., sl]
        lo = jnp.maximum(hi - w, 0)
        win = jnp.take(csg, hi, axis=1) - jnp.take(csg, lo, axis=1)
        cnt = jnp.minimum(pos + 1, w).astype(jnp.float32)
        pooled = (win / cnt[None, :, None] - cur[..., sl]).astype(a_ext.dtype)
        outs.append(pooled @ pool_w[g])
    return jnp.concatenate(outs, axis=-1) * pool_scale


def fox_attend(q, k, v, Fq, Fk, q_pos, k_pos):
    s = jnp.einsum('bqhd,bkhd->bhqk', q, k).astype(jnp.float32) * (HEAD_DIM ** -0.5)
    s = s + jnp.transpose(Fq, (0, 2, 1))[..., :, None] - jnp.transpose(Fk, (0, 2, 1))[..., None, :]
    s = jnp.where(k_pos[None, :] <= q_pos[:, None], s, -jnp.inf)
    p = jax.nn.softmax(s, axis=-1).astype(v.dtype)
    return jnp.einsum('bhqk,bkhd->bqhd', p, v)


def fox_prompt(q, k, v, logf):
    B, T, H, Dh = q.shape
    F = jnp.cumsum(logf, axis=1)
    nb = T // Q_BLOCK
    qb = q.reshape(B, nb, Q_BLOCK, H, Dh).transpose(1, 0, 2, 3, 4)
    Fb = F.reshape(B, nb, Q_BLOCK, H).transpose(1, 0, 2, 3)
    starts = jnp.arange(nb) * Q_BLOCK
    k_pos = jnp.arange(T)

    def block(args):
        qi, Fi, st = args
        return fox_attend(qi, k, v, Fi, F, st + jnp.arange(Q_BLOCK), k_pos)

    o = lax.map(block, (qb, Fb, starts))
    return o.transpose(1, 0, 2, 3, 4).reshape(B, T, H, Dh)


def fox_sample(q, k_new, v_new, logf_new, k_past, v_past, logf_past):
    P = k_past.shape[1]
    T = q.shape[1]
    F = jnp.cumsum(jnp.concatenate([logf_past.astype(jnp.float32), logf_new], axis=1), axis=1)
    s = jnp.concatenate([jnp.einsum('bqhd,bkhd->bhqk', q, k_past),
                         jnp.einsum('bqhd,bkhd->bhqk', q, k_new)], axis=-1).astype(jnp.float32)
    s = s * (HEAD_DIM ** -0.5)
    s = s + jnp.transpose(F[:, P:], (0, 2, 1))[..., :, None] - jnp.transpose(F, (0, 2, 1))[..., None, :]
    q_pos = P + jnp.arange(T)
    k_pos = jnp.arange(P + T)
    s = jnp.where(k_pos[None, :] <= q_pos[:, None], s, -jnp.inf)
    p = jax.nn.softmax(s, axis=-1).astype(v_new.dtype)
    return (jnp.einsum('bhqk,bkhd->bqhd', p[..., :P], v_past)
            + jnp.einsum('bhqk,bkhd->bqhd', p[..., P:], v_new))


def even_mixer(x, pool_hist, conv_hist, first_pos, w_in, pool_w, pool_scale, conv_w, conv_bias, ln_g, ln_b, w_out):
    proj = x @ w_in
    a = proj[..., :A_WIDTH]
    val = proj[..., A_WIDTH:A_WIDTH + B_WIDTH]
    gate = proj[..., A_WIDTH + B_WIDTH:]
    u = val * jax.nn.sigmoid(gate)
    if pool_hist is None:
        a_ext, n_hist = a, 0
    else:
        a_ext, n_hist = jnp.concatenate([pool_hist, a], axis=1), pool_hist.shape[1]
    y_a = multiscale_pool(a_ext, n_hist, first_pos, pool_w, pool_scale)
    if conv_hist is None:
        conv_hist = jnp.zeros((x.shape[0], CONV_B_WIDTH - 1, B_WIDTH), u.dtype)
    u_ext = jnp.concatenate([conv_hist, u], axis=1)
    y_b = jax.nn.silu(layer_norm(causal_dwconv(u_ext, conv_w) + conv_bias, ln_g, ln_b))
    out = jnp.concatenate([y_a, y_b], axis=-1) @ w_out
    return out, a_ext[:, -POOL_HIST:], u_ext[:, -(CONV_B_WIDTH - 1):]


def odd_mixer(x, past, conv_hist, w_in, forget_bias, conv_w, w_out):
    Bsz, T, _ = x.shape
    proj = x @ w_in
    cuts = [C_WIDTH, 2 * C_WIDTH, 3 * C_WIDTH, 3 * C_WIDTH + N_HEADS,
            3 * C_WIDTH + N_HEADS + D_WIDTH, 3 * C_WIDTH + N_HEADS + 2 * D_WIDTH]
    q, k, v, fl, h, bg, cg = jnp.split(proj, cuts, axis=-1)
    q = q.reshape(Bsz, T, N_HEADS, HEAD_DIM)
    k = k.reshape(Bsz, T, N_HEADS, HEAD_DIM)
    v = v.reshape(Bsz, T, N_HEADS, HEAD_DIM)
    logf = jax.nn.log_sigmoid(fl.astype(jnp.float32) + forget_bias.astype(jnp.float32))
    if past is None:
        o = fox_prompt(q, k, v, logf)
        conv_hist = jnp.zeros((Bsz, CONV_D_WIDTH - 1, D_WIDTH), h.dtype)
    else:
        o = fox_sample(q, k, v, logf, *past)
    u_ext = jnp.concatenate([conv_hist, cg * h], axis=1)
    y_d = bg * causal_dwconv(u_ext, conv_w)
    out = jnp.concatenate([o.reshape(Bsz, T, C_WIDTH), y_d], axis=-1) @ w_out
    return out, k, v, logf.astype(x.dtype), u_ext[:, -(CONV_D_WIDTH - 1):]


def swiglu(x, w1, w3, w2):
    return (jax.nn.silu(x @ w1) * (x @ w3)) @ w2


def moe_swiglu(x, router_w, w1, w3, w2):
    logits = (x @ router_w).astype(jnp.float32)
    top_v, top_i = lax.top_k(logits, TOP_K)
    gates = jax.nn.softmax(top_v, axis=-1)
    dense_gate = jnp.sum(jax.nn.one_hot(top_i, N_EXPERTS, dtype=jnp.float32) * gates[..., None], axis=-2)
    dense_gate = dense_gate.astype(x.dtype)
    out = jnp.zeros_like(x)
    for e in range(N_EXPERTS):
        out = out + dense_gate[..., e:e + 1] * swiglu(x, w1[e], w3[e], w2[e])
    return out


def setup_inputs(seed: int = 0) -> dict:
    key = jax.random.key(seed)
    ks = iter(jax.random.split(key, 64))

    def nrm(shape, scale):
        return jax.random.normal(next(ks), shape, jnp.float32) * scale

    n_pages = PAST_LEN // PAGE_SIZE
    n_pool = (DEC_BATCH * n_pages * 5) // 4
    page_table = jax.random.permutation(next(ks), n_pool)[:DEC_BATCH * n_pages]
    page_table = page_table.reshape(DEC_BATCH, n_pages).astype(jnp.int32)
    return {
        'x_prompt': nrm((BATCH, SEQ, D_MODEL), 1.0),
        'x_sample': nrm((DEC_BATCH, DEC_SEQ, D_MODEL), 1.0),
        'state_pool': nrm((N_EVEN, DEC_BATCH, POOL_HIST, A_WIDTH), 1.0),
        'state_conv_b': nrm((N_EVEN, DEC_BATCH, CONV_B_WIDTH - 1, B_WIDTH), 0.5),
        'cache_k': nrm((N_ODD, n_pool, PAGE_SIZE, N_HEADS, HEAD_DIM), 1.0),
        'cache_v': nrm((N_ODD, n_pool, PAGE_SIZE, N_HEADS, HEAD_DIM), 1.0),
        'cache_logf': jax.nn.log_sigmoid(FORGET_BIAS + nrm((N_ODD, n_pool, PAGE_SIZE, N_HEADS), 1.0)),
        'state_conv_d': nrm((N_ODD, DEC_BATCH, CONV_D_WIDTH - 1, D_WIDTH), 0.5),
        'page_table': page_table,
        'w_in_even': nrm((N_EVEN, D_MODEL, EVEN_IN), D_MODEL ** -0.5),
        'pool_w': nrm((N_EVEN, len(POOL_WINDOWS), POOL_GROUP, POOL_GROUP), POOL_GROUP ** -0.5),
        'pool_scale': 1.0 + nrm((N_EVEN, A_WIDTH), 0.1),
        'conv_b_w': nrm((N_EVEN, CONV_B_WIDTH, B_WIDTH), CONV_B_WIDTH ** -0.5),
        'conv_b_bias': nrm((N_EVEN, B_WIDTH), 0.01),
        'conv_ln_g': 1.0 + nrm((N_EVEN, B_WIDTH), 0.05),
        'conv_ln_b': nrm((N_EVEN, B_WIDTH), 0.01),
        'w_out_even': nrm((N_EVEN, MIX_WIDTH, D_MODEL), BETA * MIX_WIDTH ** -0.5),
        'ln_mix_even_g': 1.0 + nrm((N_EVEN, D_MODEL), 0.05),
        'ln_mix_even_b': nrm((N_EVEN, D_MODEL), 0.01),
        'ffn_w1': nrm((N_EVEN, D_MODEL, D_FF), D_MODEL ** -0.5),
        'ffn_w3': nrm((N_EVEN, D_MODEL, D_FF), D_MODEL ** -0.5),
        'ffn_w2': nrm((N_EVEN, D_FF, D_MODEL), BETA * D_FF ** -0.5),
        'ln_ffn_even_g': 1.0 + nrm((N_EVEN, D_MODEL), 0.05),
        'ln_ffn_even_b': nrm((N_EVEN, D_MODEL), 0.01),
        'w_in_odd': nrm((N_ODD, D_MODEL, ODD_IN), D_MODEL ** -0.5),
        'forget_bias': FORGET_BIAS + nrm((N_ODD, N_HEADS), 0.5),
        'conv_d_w': nrm((N_ODD, CONV_D_WIDTH, D_WIDTH), CONV_D_WIDTH ** -0.5),
        'w_out_odd': nrm((N_ODD, MIX_WIDTH, D_MODEL), BETA * MIX_WIDTH ** -0.5),
        'ln_mix_odd_g': 1.0 + nrm((N_ODD, D_MODEL), 0.05),
        'ln_mix_odd_b': nrm((N_ODD, D_MODEL), 0.01),
        'router_w': nrm((N_ODD, D_MODEL, N_EXPERTS), D_MODEL ** -0.5),
        'moe_w1': nrm((N_ODD, N_EXPERTS, D_MODEL, D_FF_EXPERT), D_MODEL ** -0.5),
        'moe_w3': nrm((N_ODD, N_EXPERTS, D_MODEL, D_FF_EXPERT), D_MODEL ** -0.5),
        'moe_w2': nrm((N_ODD, N_EXPERTS, D_FF_EXPERT, D_MODEL), BETA * D_FF_EXPERT ** -0.5),
        'ln_ffn_odd_g': 1.0 + nrm((N_ODD, D_MODEL), 0.05),
        'ln_ffn_odd_b': nrm((N_ODD, D_MODEL), 0.01),
    }


def reference(x_prompt, x_sample, state_pool, state_conv_b, cache_k, cache_v, cache_logf, state_conv_d,
              page_table, w_in_even, pool_w, pool_scale, conv_b_w, conv_b_bias, conv_ln_g, conv_ln_b,
              w_out_even, ln_mix_even_g, ln_mix_even_b, ffn_w1, ffn_w3, ffn_w2, ln_ffn_even_g, ln_ffn_even_b,
              w_in_odd, forget_bias, conv_d_w, w_out_odd, ln_mix_odd_g, ln_mix_odd_b, router_w,
              moe_w1, moe_w3, moe_w2, ln_ffn_odd_g, ln_ffn_odd_b):
    dec_b = page_table.shape[0]
    past_len = page_table.shape[1] * cache_k.shape[2]
    xp, xs = x_prompt, x_sample
    pool_p, pool_s, convb_p, convb_s = [], [], [], []
    k_p, k_s, v_p, v_s, lf_p, lf_s, convd_p, convd_s = [], [], [], [], [], [], [], []
    for layer in range(DEPTH):
        j = layer // 2
        if layer % 2 == 0:
            ew = (w_in_even[j], pool_w[j], pool_scale[j], conv_b_w[j], conv_b_bias[j],
                  conv_ln_g[j], conv_ln_b[j], w_out_even[j])
            mp, sp_pool, sp_conv = even_mixer(xp, None, None, 0, *ew)
            ms, ss_pool, ss_conv = even_mixer(xs, state_pool[j], state_conv_b[j], past_len, *ew)
            pool_p.append(sp_pool); pool_s.append(ss_pool)
            convb_p.append(sp_conv); convb_s.append(ss_conv)
            xp = layer_norm(ALPHA * xp + mp, ln_mix_even_g[j], ln_mix_even_b[j])
            xs = layer_norm(ALPHA * xs + ms, ln_mix_even_g[j], ln_mix_even_b[j])
            xp = layer_norm(ALPHA * xp + swiglu(xp, ffn_w1[j], ffn_w3[j], ffn_w2[j]), ln_ffn_even_g[j], ln_ffn_even_b[j])
            xs = layer_norm(ALPHA * xs + swiglu(xs, ffn_w1[j], ffn_w3[j], ffn_w2[j]), ln_ffn_even_g[j], ln_ffn_even_b[j])
        else:
            k_past = cache_k[j][page_table].reshape(dec_b, past_len, N_HEADS, HEAD_DIM)
            v_past = cache_v[j][page_table].reshape(dec_b, past_len, N_HEADS, HEAD_DIM)
            lf_past = cache_logf[j][page_table].reshape(dec_b, past_len, N_HEADS)
            ow = (w_in_odd[j], forget_bias[j], conv_d_w[j], w_out_odd[j])
            mp, kp, vp, lp, cp = odd_mixer(xp, None, None, *ow)
            ms, ks_, vs_, ls_, cs_ = odd_mixer(xs, (k_past, v_past, lf_past), state_conv_d[j], *ow)
            k_p.append(kp); k_s.append(ks_); v_p.append(vp); v_s.append(vs_)
            lf_p.append(lp); lf_s.append(ls_); convd_p.append(cp); convd_s.append(cs_)
            xp = layer_norm(ALPHA * xp + mp, ln_mix_odd_g[j], ln_mix_odd_b[j])
            xs = layer_norm(ALPHA * xs + ms, ln_mix_odd_g[j], ln_mix_odd_b[j])
            xp = layer_norm(ALPHA * xp + moe_swiglu(xp, router_w[j], moe_w1[j], moe_w3[j], moe_w2[j]), ln_ffn_odd_g[j], ln_ffn_odd_b[j])
            xs = layer_norm(ALPHA * xs + moe_swiglu(xs, router_w[j], moe_w1[j], moe_w3[j], moe_w2[j]), ln_ffn_odd_g[j], ln_ffn_odd_b[j])
    return (xp, xs,
            jnp.stack(pool_p), jnp.stack(pool_s), jnp.stack(convb_p), jnp.stack(convb_s),
            jnp.stack(k_p), jnp.stack(k_s), jnp.stack(v_p), jnp.stack(v_s),
            jnp.stack(lf_p), jnp.stack(lf_s), jnp.stack(convd_p), jnp.stack(convd_s))
```

```python
import numpy as np
from contextlib import ExitStack
import concourse.bass as bass
import concourse.mybir as mybir
from concourse.bass_utils import run_bass_kernel_spmd

F32 = mybir.dt.float32
BF16 = mybir.dt.bfloat16
I32 = mybir.dt.int32
ALU = mybir.AluOpType
AF = mybir.ActivationFunctionType
AX = mybir.AxisListType

D = 1024
SEQ = 4096
HALF = 2048
NS = 4
DFF = 2816
NE = 8
DFE = 3584
ODD_IN = 3080
ALPHA = float(4 ** 0.25)
EPS = 1e-5
NEG = -30000.0
NPAGES = 64


class Buf:
    __slots__ = ("t", "w", "r")

    def __init__(self, t):
        self.t = t
        self.w = None
        self.r = {}

    def __getitem__(self, k):
        return self.t[k]


class Sync:
    LIMIT = 30000

    def __init__(self, nc, es):
        self.nc = nc
        self.es = es
        self.engs = {"pe": nc.tensor, "act": nc.scalar, "dve": nc.vector, "pool": nc.gpsimd, "sp": nc.sync}
        self.sems = []
        self.cur = {}
        self.cnt = {}
        self.seen = {e: {} for e in self.engs}
        for e in self.engs:
            self._new_sem(e)
        self.dsem = []
        self.dval = []
        for i in range(24):
            self.sems.append(es.enter_context(nc.semaphore("d%d" % i)))
            self.dsem.append(len(self.sems) - 1)
            self.dval.append(0)
        self.dnext = 0
        self.last = {}

    def _new_sem(self, e):
        self.sems.append(self.es.enter_context(self.nc.semaphore("s%s%d" % (e, len(self.sems)))))
        self.cur[e] = len(self.sems) - 1
        self.cnt[e] = 0

    def wait(self, e, tk):
        if tk is None:
            return
        k, v = tk
        if self.seen[e].get(k, 0) >= v:
            return
        if k == self.cur.get(e) and e == "pe":
            return
        self.engs[e].wait_ge(self.sems[k], v)
        self.seen[e][k] = v

    def _deps(self, e, reads, writes):
        for b in reads:
            self.wait(e, b.w)
        for b in writes:
            self.wait(e, b.w)
            for t in b.r.values():
                self.wait(e, t)

    def _post(self, key, tk, reads, writes):
        for b in reads:
            b.r[key] = tk
        for b in writes:
            b.w = tk
            b.r = {}

    def op(self, e, fn, reads=(), writes=()):
        self._deps(e, reads, writes)
        if self.cnt[e] >= self.LIMIT:
            self._new_sem(e)
        ins = fn()
        self.cnt[e] += 1
        ins.then_inc(self.sems[self.cur[e]], 1)
        tk = (self.cur[e], self.cnt[e])
        self.last[e] = tk
        self._post(e, tk, reads, writes)
        return tk

    def dma(self, e, fn, reads=(), writes=()):
        self._deps(e, reads, writes)
        i = self.dnext
        self.dnext = (self.dnext + 1) % len(self.dsem)
        k = self.dsem[i]
        if self.dval[i] >= self.LIMIT:
            self.wait(e, (k, self.dval[i]))
            self.sems.append(self.es.enter_context(self.nc.semaphore("dd%d" % len(self.sems))))
            self.dsem[i] = len(self.sems) - 1
            self.dval[i] = 0
            k = self.dsem[i]
        if self.dval[i] > 0:
            self.wait(e, (k, self.dval[i]))
        ins = fn()
        self.dval[i] += 16
        ins.then_inc(self.sems[k], 16)
        tk = (k, self.dval[i])
        self.last[("d", i)] = tk
        self._post(("d", i), tk, reads, writes)
        return tk

    def barrier(self):
        tks = list(self.last.values())
        for e in self.engs:
            for tk in tks:
                self.wait(e, tk)


def build(n_pool=2560, dev=False):
    nc = bass.Bass("TRN2", target_bir_lowering=False)
    es = ExitStack()
    with es:
        _build(nc, es, n_pool, dev)
    return nc


def _build(nc, es, n_pool, dev):
    S = Sync(nc, es)
    PE, ACT, DVE, POOL, SP = nc.tensor, nc.scalar, nc.vector, nc.gpsimd, nc.sync

    def din(name, shape, dt=F32):
        return nc.dram_tensor(name, list(shape), dt, kind="ExternalInput").ap()

    def dout(name, shape, dt=F32):
        return nc.dram_tensor(name, list(shape), dt, kind="ExternalOutput").ap()

    def dscr(name, shape, dt=F32):
        return nc.dram_tensor(name, list(shape), dt, kind=("ExternalOutput" if dev else "Internal")).ap()

    def sb(st, name, shape, dt=F32):
        return Buf(st.enter_context(nc.sbuf_tensor(name, list(shape), dt)))

    def ps(st, name, shape, dt=F32):
        return Buf(st.enter_context(nc.psum_tensor(name, list(shape), dt)))

    xloc = din("xloc", [SEQ, D]); xs = din("xs", [NS, D])
    spool = din("spool", [NS * 15, 512]); sconvb = din("sconvb", [NS * 30, 512]); sconvd = din("sconvd", [NS * 2, 512])
    cache_k = din("cache_k", [n_pool * 128, 512]); cache_v = din("cache_v", [n_pool * 128, 512])
    cache_lf = din("cache_lf", [n_pool * 128, 8]); ptab = din("ptab", [NS, NPAGES], I32)
    pmask = din("pmask", [128, 32]); invcnt = din("invcnt", [4, SEQ]); hflag = din("hflag", [128, 1])
    w_in_even = din("w_in_even", [D, 1536]); pool_w = din("pool_w", [4, 128, 128]); pool_scale = din("pool_scale", [512])
    conv_b_w = din("conv_b_w", [31, 512]); conv_b_bias = din("conv_b_bias", [512])
    conv_ln_g = din("conv_ln_g", [512]); conv_ln_b = din("conv_ln_b", [512])
    w_out_even = din("w_out_even", [D, D]); ln_mix_even_g = din("ln_mix_even_g", [D]); ln_mix_even_b = din("ln_mix_even_b", [D])
    ffn_w1 = din("ffn_w1", [D, DFF]); ffn_w3 = din("ffn_w3", [D, DFF]); ffn_w2 = din("ffn_w2", [DFF, D])
    ln_ffn_even_g = din("ln_ffn_even_g", [D]); ln_ffn_even_b = din("ln_ffn_even_b", [D])
    w_in_odd = din("w_in_odd", [D, ODD_IN]); forget_bias = din("forget_bias", [8]); conv_d_w = din("conv_d_w", [3, 512])
    w_out_odd = din("w_out_odd", [D, D]); ln_mix_odd_g = din("ln_mix_odd_g", [D]); ln_mix_odd_b = din("ln_mix_odd_b", [D])
    router_w = din("router_w", [D, NE]); moe_w1 = din("moe_w1", [NE, D, DFE]); moe_w3 = din("moe_w3", [NE, D, DFE])
    moe_w2 = din("moe_w2", [NE, DFE, D]); ln_ffn_odd_g = din("ln_ffn_odd_g", [D]); ln_ffn_odd_b = din("ln_ffn_odd_b", [D])

    y_own = dout("y_own", [HALF, D]); y_s = dout("y_s", [NS, D])
    pool_tail = dout("pool_tail", [15, 512]); pool_s = dout("pool_s", [NS, 15, 512])
    convb_tail = dout("convb_tail", [30, 512]); convb_s = dout("convb_s", [NS, 30, 512])
    k_own = dout("k_own", [HALF, 512]); k_s = dout("k_s", [NS, 512])
    v_own = dout("v_own", [HALF, 512]); v_s = dout("v_s", [NS, 512])
    lf_own = dout("lf_own", [HALF, 8]); lf_s = dout("lf_s", [NS, 8])
    convd_tail = dout("convd_tail", [2, 512]); convd_s = dout("convd_s", [NS, 2, 512])

    xm_scr = dscr("xm_scr", [SEQ + NS, D]); x1_scr = dscr("x1_scr", [SEQ + NS, D]); x2_scr = dscr("x2_scr", [HALF + NS, D])

    G = es
    ident_f = sb(G, "ident_f", [128, 128]); ident_b = sb(G, "ident_b", [128, 128], BF16)
    ones_f = sb(G, "ones_f", [128, 128]); epsb = sb(G, "epsb", [128, 1])
    S.op("pool", lambda: POOL.memset(ones_f[:], 1.0), writes=[ones_f])
    S.op("pool", lambda: POOL.memset(epsb[:], EPS), writes=[epsb])
    S.op("pool", lambda: POOL.affine_select(out=ident_f[:], in_=ones_f[:], pattern=[[1, 128]], compare_op=ALU.is_equal,
                                             fill=0.0, base=0, channel_multiplier=-1), reads=[ones_f], writes=[ident_f])
    S.op("pool", lambda: POOL.tensor_copy(out=ident_b[:], in_=ident_f[:]), reads=[ident_f], writes=[ident_b])

    pbank = [ps(G, "pb%d" % i, [128, 512]) for i in range(7)]
    ptr = ps(G, "ptr", [128, 1024], BF16)
    pb_i = [0]

    def bank():
        b = pbank[pb_i[0] % 7]
        pb_i[0] += 1
        return b

    def load_w_bf(dst, dst_ap, src_ap):
        S.dma("pool", lambda: POOL.dma_start(out=dst_ap, in_=src_ap), writes=[dst])

    def load(eng, dst, dst_ap, src_ap):
        e = S.engs[eng]
        S.dma(eng, lambda: e.dma_start(out=dst_ap, in_=src_ap), writes=[dst])

    def store(eng, src, dst_ap, src_ap):
        e = S.engs[eng]
        return S.dma(eng, lambda: e.dma_start(out=dst_ap, in_=src_ap), reads=[src])

    def mm(pbuf, out_ap, pairs, reads):
        n = len(pairs)
        for i, (l, r) in enumerate(pairs):
            S.op("pe", lambda l=l, r=r, i=i: PE.matmul(out_ap, lhsT=l, rhs=r, start=(i == 0), stop=(i == n - 1)),
                 reads=reads, writes=[pbuf])

    def transpose(pbuf, out_ap, in_ap, idn, reads):
        S.op("pe", lambda: PE.transpose(out=out_ap, in_=in_ap, identity=idn), reads=reads, writes=[pbuf])

    def layernorm_rows(st_, r, nrows, gB, bB, out, tag):
        stats = st_["stats"]; mv = st_["mv"]; rstd = st_["rstd"]
        for h in range(2):
            S.op("dve", lambda h=h: DVE.bn_stats(out=stats[:nrows, h, :], in_=r[:nrows, h * 512:(h + 1) * 512]),
                 reads=[r], writes=[stats])
        S.op("dve", lambda: DVE.bn_aggr(out=mv[:nrows, :], in_=stats[:nrows, :, :]), reads=[stats], writes=[mv])
        S.op("act", lambda: ACT.activation(out=rstd[:nrows, :], in_=mv[:nrows, 1:2], func=AF.Sqrt, bias=epsb[:nrows, :], scale=1.0),
             reads=[mv, epsb], writes=[rstd])
        S.op("dve", lambda: DVE.reciprocal(out=rstd[:nrows, :], in_=rstd[:nrows, :]), reads=[rstd], writes=[rstd])
        S.op("dve", lambda: DVE.tensor_scalar(out=out[:nrows, :], in0=r[:nrows, :], scalar1=mv[:nrows, 0:1], scalar2=rstd[:nrows, 0:1],
                                              op0=ALU.subtract, op1=ALU.mult), reads=[r, mv, rstd], writes=[out])
        S.op("pool", lambda: POOL.tensor_tensor(out=out[:nrows, :], in0=out[:nrows, :], in1=gB[:nrows, :], op=ALU.mult),
             reads=[out, gB], writes=[out])
        S.op("pool", lambda: POOL.tensor_tensor(out=out[:nrows, :], in0=out[:nrows, :], in1=bB[:nrows, :], op=ALU.add),
             reads=[out, bB], writes=[out])

    def to_fm(xtok, nrows, xbf, xT, col0, ncols_total_view=None):
        S.op("pool", lambda: POOL.tensor_copy(out=xbf[:nrows, :], in_=xtok[:nrows, :]), reads=[xtok], writes=[xbf])
        for k in range(8):
            transpose(ptr, ptr[:, k * 128:k * 128 + nrows], xbf[:nrows, k * 128:(k + 1) * 128], ident_b[:nrows, :nrows], [xbf, ident_b])
        S.op("act", lambda: ACT.copy(out=xT[:, :, col0:col0 + nrows],
                                     in_=ptr[:, :].rearrange("p (k t) -> p k t", k=8)[:, :, 0:nrows]), reads=[ptr], writes=[xT])

    lnst = {"stats": sb(G, "ln_stats", [128, 2, 6]), "mv": sb(G, "ln_mv", [128, 2]), "rstd": sb(G, "ln_rstd", [128, 1])}

    with ExitStack() as P1:
        w_in = sb(P1, "w_in", [128, 8, 1536], BF16)
        w_out = sb(P1, "w_out", [128, 8, 1024], BF16)
        poolw = sb(P1, "poolw", [128, 4, 128], BF16)
        load_w_bf(w_in, w_in[:], w_in_even.rearrange("(c p) n -> p c n", p=128))
        load_w_bf(w_out, w_out[:], w_out_even.rearrange("(c p) n -> p c n", p=128))
        load_w_bf(poolw, poolw[:], pool_w.rearrange("g k m -> k g m"))
        vecs = sb(P1, "vecs", [128, 4, 4])
        for i, v in enumerate((pool_scale, conv_b_bias, conv_ln_g, conv_ln_b)):
            S.dma("sp", lambda i=i, v=v: SP.dma_start(out=vecs[:, i, :], in_=v.rearrange("(c p) -> p c", p=128),
                                                        allow_slow_non_contiguous=True), writes=[vecs])
        gB = sb(P1, "gB", [128, D]); bB = sb(P1, "bB", [128, D])
        load("sp", gB, gB[:], ln_mix_even_g.partition_broadcast(128))
        load("sp", bB, bB[:], ln_mix_even_b.partition_broadcast(128))
        cw_tok = sb(P1, "cw_tok", [31, 512]); convw = sb(P1, "convw", [128, 4, 31])
        load("sp", cw_tok, cw_tok[:], conv_b_w)
        pb = bank()
        for c in range(4):
            transpose(pb, pb[:, c * 31:(c + 1) * 31], cw_tok[:, c * 128:(c + 1) * 128], ident_f[:31, :31], [cw_tok, ident_f])
        S.op("dve", lambda: DVE.tensor_copy(out=convw[:], in_=pb[:, 0:124].rearrange("p (c j) -> p c j", c=4)),
             reads=[pb], writes=[convw])
        diag = sb(P1, "diag", [128, 4, 31, 128], BF16)
        for c in range(4):
            for j in range(31):
                S.op("dve", lambda c=c, j=j: DVE.tensor_scalar(out=diag[:, c, j, :], in0=ident_f[:], scalar1=convw[:, c, j:j + 1],
                                                               scalar2=None, op0=ALU.mult), reads=[ident_f, convw], writes=[diag])

        xtok = sb(P1, "xtok", [128, 4, D]); xbf = sb(P1, "xbf", [128, D], BF16)
        xT = sb(P1, "xT", [128, 8, 512], BF16)
        aT = sb(P1, "aT", [128, 4, 528]); u32 = sb(P1, "u32", [128, 4, 544]); ubf = sb(P1, "ubf", [128, 4, 544], BF16)
        sg = sb(P1, "sg", [128, 512]); t1 = sb(P1, "t1", [128, 528]); t2 = sb(P1, "t2", [128, 528])
        invc = sb(P1, "invc", [128, 4, 512]); pooled = sb(P1, "pooled", [128, 512], BF16)
        yT = sb(P1, "yT", [128, 8, 512], BF16)
        cf = sb(P1, "cf", [128, 4, 512]); sq = sb(P1, "sq", [128, 4, 512])
        mean = sb(P1, "mean", [128, 512]); rs = sb(P1, "rs", [128, 512]); tmp = sb(P1, "tmp", [128, 512])
        r = sb(P1, "r", [128, D]); xo = sb(P1, "xo", [128, D])
        for b_ in (aT, u32, ubf):
            S.op("pool", lambda b_=b_: POOL.memset(b_[:], 0.0), writes=[b_])

        def mixer_core(ncol, xTv, a_dst, sg_v, u_dst, ub_dst):
            for m in list(range(0, 4)) + [x for c in range(4) for x in (8 + c, 4 + c)]:
                pb = bank()
                mm(pb, pb[:, :ncol], [(w_in[:, k, m * 128:(m + 1) * 128], xTv(k)) for k in range(8)], [w_in, xT_cur[0]])
                if m < 4:
                    S.op("act", lambda m=m, pb=pb: ACT.copy(out=a_dst(m), in_=pb[:, :ncol]), reads=[pb], writes=[a_cur[0]])
                elif m >= 8:
                    S.op("act", lambda pb=pb: ACT.activation(out=sg_v, in_=pb[:, :ncol], func=AF.Sigmoid), reads=[pb], writes=[sg])
                else:
                    c = m - 4
                    S.op("dve", lambda c=c, pb=pb: DVE.tensor_tensor(out=u_dst(c), in0=pb[:, :ncol], in1=sg_v, op=ALU.mult),
                         reads=[pb, sg], writes=[u_cur[0]])
                    if ub_dst is not None:
                        S.op("pool", lambda c=c: POOL.tensor_copy(out=ub_dst(c), in_=u_dst(c)), reads=[u_cur[0]], writes=[ubf])

        def conv_ln_silu(ncol, cfv, sqv, y_dst, ybuf):
            for c in range(4):
                S.op("act", lambda c=c: ACT.activation(out=sqv(c), in_=cfv(c), func=AF.Square), reads=[cf], writes=[sq])
            p1 = bank(); p2 = bank()
            mm(p1, p1[:, :ncol], [(ones_f[:], cfv(c)) for c in range(4)], [ones_f, cf])
            mm(p2, p2[:, :ncol], [(ones_f[:], sqv(c)) for c in range(4)], [ones_f, sq])
            S.op("act", lambda: ACT.mul(out=mean[:, :ncol], in_=p1[:, :ncol], mul=1.0 / 512), reads=[p1], writes=[mean])
            S.op("dve", lambda: DVE.tensor_tensor(out=tmp[:, :ncol], in0=mean[:, :ncol], in1=mean[:, :ncol], op=ALU.mult),
                 reads=[mean], writes=[tmp])
            S.op("dve", lambda: DVE.scalar_tensor_tensor(out=rs[:, :ncol], in0=p2[:, :ncol], scalar=1.0 / 512, in1=tmp[:, :ncol],
                                                         op0=ALU.mult, op1=ALU.subtract), reads=[p2, tmp], writes=[rs])
            S.op("act", lambda: ACT.activation(out=rs[:, :ncol], in_=rs[:, :ncol], func=AF.Sqrt, bias=epsb[:, :], scale=1.0),
                 reads=[rs, epsb], writes=[rs])
            S.op("dve", lambda: DVE.reciprocal(out=rs[:, :ncol], in_=rs[:, :ncol]), reads=[rs], writes=[rs])
            for c in range(4):
                S.op("dve", lambda c=c: DVE.tensor_tensor(out=tmp[:, :ncol], in0=cfv(c), in1=mean[:, :ncol], op=ALU.subtract),
                     reads=[cf, mean], writes=[tmp])
                S.op("dve", lambda: DVE.tensor_tensor(out=tmp[:, :ncol], in0=tmp[:, :ncol], in1=rs[:, :ncol], op=ALU.mult),
                     reads=[tmp, rs], writes=[tmp])
                S.op("act", lambda c=c: ACT.activation(out=y_dst(c), in_=tmp[:, :ncol], func=AF.Silu,
                                                       bias=vecs[:, 3, c:c + 1], scale=vecs[:, 2, c:c + 1]),
                     reads=[tmp, vecs], writes=[ybuf])

        xT_cur = [xT]; a_cur = [aT]; u_cur = [u32]
        for it in range(SEQ // 512):
            t0 = it * 512
            for tt in range(4):
                load("sp", xtok, xtok[:, tt, :], xloc[t0 + tt * 128:t0 + (tt + 1) * 128, :])
            for g in range(4):
                load("sp", invc, invc[:, g, :], invcnt[g, t0:t0 + 512].partition_broadcast(128))
            xtk = [Buf(xtok.t) for _ in range(4)]
            for tt in range(4):
                S.op("pool", lambda tt=tt: POOL.tensor_copy(out=xbf[:, :], in_=xtok[:, tt, :]), reads=[xtok], writes=[xbf])
                for k in range(8):
                    transpose(ptr, ptr[:, k * 128:(k + 1) * 128], xbf[:, k * 128:(k + 1) * 128], ident_b[:], [xbf, ident_b])
                S.op("act", lambda tt=tt: ACT.copy(out=xT[:, :, tt * 128:(tt + 1) * 128],
                                                   in_=ptr[:, :].rearrange("p (k t) -> p k t", k=8)), reads=[ptr], writes=[xT])
            mixer_core(512, lambda k: xT[:, k, :], lambda m: aT[:, m, 16:528], sg[:, :], lambda c: u32[:, c, 32:544],
                       lambda c: ubf[:, c, 32:544])
            for g in range(4):
                A = aT
                S.op("dve", lambda g=g: DVE.tensor_tensor(out=t1[:, 1:528], in0=aT[:, g, 1:528], in1=aT[:, g, 0:527], op=ALU.add),
                     reads=[aT], writes=[t1])
                win = t1
                if g >= 1:
                    S.op("dve", lambda: DVE.tensor_tensor(out=t2[:, 3:528], in0=t1[:, 3:528], in1=t1[:, 1:526], op=ALU.add),
                         reads=[t1], writes=[t2])
                    win = t2
                if g >= 2:
                    S.op("dve", lambda: DVE.tensor_tensor(out=t1[:, 7:528], in0=t2[:, 7:528], in1=t2[:, 3:524], op=ALU.add),
                         reads=[t2], writes=[t1])
                    win = t1
                if g >= 3:
                    S.op("dve", lambda: DVE.tensor_tensor(out=t2[:, 15:528], in0=t1[:, 15:528], in1=t1[:, 7:520], op=ALU.add),
                         reads=[t1], writes=[t2])
                    win = t2
                S.op("dve", lambda g=g, win=win: DVE.tensor_tensor(out=tmp[:, :], in0=win[:, 16:528], in1=invc[:, g, :], op=ALU.mult),
                     reads=[win, invc], writes=[tmp])
                S.op("dve", lambda g=g: DVE.tensor_tensor(out=pooled[:, :], in0=tmp[:, :], in1=aT[:, g, 16:528], op=ALU.subtract),
                     reads=[tmp, aT], writes=[pooled])
                pb = bank()
                mm(pb, pb[:, :], [(poolw[:, g, :], pooled[:, :])], [poolw, pooled])
                S.op("act", lambda g=g, pb=pb: ACT.activation(out=yT[:, g, :], in_=pb[:, :], func=AF.Identity, scale=vecs[:, 0, g:g + 1]),
                     reads=[pb, vecs], writes=[yT])
            for c in range(4):
                pb = bank()
                mm(pb, pb[:, :], [(diag[:, c, j, :], ubf[:, c, 2 + j:2 + j + 512]) for j in range(31)], [diag, ubf])
                S.op("act", lambda c=c, pb=pb: ACT.activation(out=cf[:, c, :], in_=pb[:, :], func=AF.Identity,
                                                              bias=vecs[:, 1, c:c + 1], scale=1.0), reads=[pb, vecs], writes=[cf])
            conv_ln_silu(512, lambda c: cf[:, c, :], lambda c: sq[:, c, :], lambda c: yT[:, 4 + c, :], yT)
            for tt in range(4):
                p1 = bank(); p2 = bank()
                for hf, pb in ((0, p1), (1, p2)):
                    mm(pb, pb[:, :], [(yT[:, k, tt * 128:(tt + 1) * 128], w_out[:, k, hf * 512:(hf + 1) * 512]) for k in range(8)],
                       [yT, w_out])
                    S.op("dve", lambda tt=tt, hf=hf, pb=pb: DVE.scalar_tensor_tensor(
                        out=r[:, hf * 512:(hf + 1) * 512], in0=xtok[:, tt, hf * 512:(hf + 1) * 512], scalar=ALPHA, in1=pb[:, :],
                        op0=ALU.mult, op1=ALU.add), reads=[xtok, pb], writes=[r])
                layernorm_rows(lnst, r, 128, gB, bB, xo, "p1")
                store("sp", xo, xm_scr[t0 + tt * 128:t0 + (tt + 1) * 128, :], xo[:, :])
            S.op("pool", lambda: POOL.tensor_copy(out=aT[:, :, 1:16], in_=aT[:, :, 513:528]), reads=[aT], writes=[aT])
            S.op("pool", lambda: POOL.tensor_copy(out=u32[:, :, 2:32], in_=u32[:, :, 514:544]), reads=[u32], writes=[u32])
            S.op("pool", lambda: POOL.tensor_copy(out=ubf[:, :, 2:32], in_=ubf[:, :, 514:544]), reads=[ubf], writes=[ubf])
        tail = sb(P1, "tail", [32, 512])
        pb = bank()
        for c in range(4):
            transpose(pb, pb[:15, c * 128:(c + 1) * 128], aT[:, c, 1:16], ident_f[:], [aT, ident_f])
        S.op("dve", lambda: DVE.tensor_copy(out=tail[:15, :], in_=pb[:15, :]), reads=[pb], writes=[tail])
        store("sp", tail, pool_tail, tail[:15, :])
        pb = bank()
        for c in range(4):
            transpose(pb, pb[:30, c * 128:(c + 1) * 128], u32[:, c, 2:32], ident_f[:], [u32, ident_f])
        S.op("dve", lambda: DVE.tensor_copy(out=tail[:30, :], in_=pb[:30, :]), reads=[pb], writes=[tail])
        store("sp", tail, convb_tail, tail[:30, :])

        xs_tok = sb(P1, "xs_tok", [NS, D]); xsT = sb(P1, "xsT", [128, 8, NS], BF16)
        aTs = sb(P1, "aTs", [128, 4, NS, 16]); uTs = sb(P1, "uTs", [128, 4, NS, 31])
        st_tok = sb(P1, "st_tok", [120, 512])
        load("sp", xs_tok, xs_tok[:], xs)
        to_fm(xs_tok, NS, xbf, xsT, 0)
        load("sp", st_tok, st_tok[:60, :], spool)
        pb = bank()
        for c in range(4):
            transpose(pb, pb[:, c * 60:(c + 1) * 60], st_tok[:60, c * 128:(c + 1) * 128], ident_f[:60, :60], [st_tok, ident_f])
        S.op("dve", lambda: DVE.tensor_copy(out=aTs[:, :, :, 0:15], in_=pb[:, 0:240].rearrange("p (c b j) -> p c b j", c=4, b=NS)),
             reads=[pb], writes=[aTs])
        load("sp", st_tok, st_tok[:120, :], sconvb)
        pb = bank()
        for c in range(4):
            transpose(pb, pb[:, c * 120:(c + 1) * 120], st_tok[:120, c * 128:(c + 1) * 128], ident_f[:120, :120], [st_tok, ident_f])
        S.op("dve", lambda: DVE.tensor_copy(out=uTs[:, :, :, 0:30], in_=pb[:, 0:480].rearrange("p (c b j) -> p c b j", c=4, b=NS)),
             reads=[pb], writes=[uTs])
        xT_cur[0] = xsT; a_cur[0] = aTs; u_cur[0] = uTs
        mixer_core(NS, lambda k: xsT[:, k, :], lambda m: aTs[:, m, :, 15], sg[:, :NS], lambda c: uTs[:, c, :, 30], None)
        yTs = sb(P1, "yTs", [128, 8, NS], BF16)
        wsum = sb(P1, "wsum", [128, NS])
        for g, w in enumerate((2, 4, 8, 16)):
            S.op("dve", lambda g=g, w=w: DVE.tensor_reduce(out=wsum[:, :], in_=aTs[:, g, :, 16 - w:16], axis=AX.X, op=ALU.add),
                 reads=[aTs], writes=[wsum])
            S.op("dve", lambda g=g, w=w: DVE.scalar_tensor_tensor(out=pooled[:, :NS], in0=wsum[:, :], scalar=1.0 / w, in1=aTs[:, g, :, 15],
                                                                   op0=ALU.mult, op1=ALU.subtract), reads=[wsum, aTs], writes=[pooled])
            pb = bank()
            mm(pb, pb[:, :NS], [(poolw[:, g, :], pooled[:, :NS])], [poolw, pooled])
            S.op("act", lambda g=g, pb=pb: ACT.activation(out=yTs[:, g, :], in_=pb[:, :NS], func=AF.Identity, scale=vecs[:, 0, g:g + 1]),
                 reads=[pb, vecs], writes=[yTs])
        prod = sb(P1, "prod", [128, NS, 31])
        for c in range(4):
            S.op("dve", lambda c=c: DVE.tensor_tensor(out=prod[:, :, :], in0=uTs[:, c, :, :],
                                                      in1=convw[:, c:c + 1, :].to_broadcast([128, NS, 31]), op=ALU.mult),
                 reads=[uTs, convw], writes=[prod])
            S.op("dve", lambda: DVE.tensor_reduce(out=wsum[:, :], in_=prod[:, :, :], axis=AX.X, op=ALU.add), reads=[prod], writes=[wsum])
            S.op("act", lambda c=c: ACT.activation(out=cf[:, c, :NS], in_=wsum[:, :], func=AF.Identity, bias=vecs[:, 1, c:c + 1], scale=1.0),
                 reads=[wsum, vecs], writes=[cf])
        conv_ln_silu(NS, lambda c: cf[:, c, :NS], lambda c: sq[:, c, :NS], lambda c: yTs[:, 4 + c, :], yTs)
        p1 = bank(); p2 = bank()
        for hf, pb in ((0, p1), (1, p2)):
            mm(pb, pb[:NS, :], [(yTs[:, k, :], w_out[:, k, hf * 512:(hf + 1) * 512]) for k in range(8)], [yTs, w_out])
            S.op("dve", lambda hf=hf, pb=pb: DVE.scalar_tensor_tensor(
                out=r[:NS, hf * 512:(hf + 1) * 512], in0=xs_tok[:, hf * 512:(hf + 1) * 512], scalar=ALPHA, in1=pb[:NS, :],
                op0=ALU.mult, op1=ALU.add), reads=[xs_tok, pb], writes=[r])
        layernorm_rows(lnst, r, NS, gB, bB, xo, "p1s")
        store("sp", xo, xm_scr[SEQ:SEQ + NS, :], xo[:NS, :])
        new_tok = sb(P1, "new_tok", [NS, 1536])
        for n3 in range(3):
            pb = bank()
            mm(pb, pb[:NS, :], [(xsT[:, k, :], w_in[:, k, n3 * 512:(n3 + 1) * 512]) for k in range(8)], [xsT, w_in])
            if n3 < 2:
                S.op("act", lambda n3=n3, pb=pb: ACT.copy(out=new_tok[:, n3 * 512:(n3 + 1) * 512], in_=pb[:NS, :]), reads=[pb], writes=[new_tok])
            else:
                S.op("act", lambda pb=pb: ACT.activation(out=new_tok[:, 1024:1536], in_=pb[:NS, :], func=AF.Sigmoid), reads=[pb], writes=[new_tok])
        S.op("dve", lambda: DVE.tensor_tensor(out=new_tok[:, 512:1024], in0=new_tok[:, 512:1024], in1=new_tok[:, 1024:1536], op=ALU.mult),
             reads=[new_tok], writes=[new_tok])
        store("sp", new_tok, pool_s[:, 14, :], new_tok[:, 0:512])
        store("sp", new_tok, convb_s[:, 29, :], new_tok[:, 512:1024])
        dummy = Buf(None)
        S.dma("sp", lambda: SP.dma_start(out=pool_s[:, 0:14, :], in_=spool.rearrange("(b j) c -> b j c", b=NS)[:, 1:15, :]))
        S.dma("sp", lambda: SP.dma_start(out=convb_s[:, 0:29, :], in_=sconvb.rearrange("(b j) c -> b j c", b=NS)[:, 1:30, :]))
        S.barrier()

    S.barrier()

    def gated_pass(name, src_rows, experts, lng, lnb, dst_rows, router=None):
        NTL = len(src_rows)
        cols = []
        c0 = 0
        for (_, n) in src_rows:
            cols.append((c0, n)); c0 += n
        NT = c0
        ntiles = [(c, min(512, NT - c)) for c in range(0, NT, 512)]
        with ExitStack() as P:
            xT2 = sb(P, name + "xT", [128, 8, NT], BF16)
            acc = sb(P, name + "acc", [128, NTL, D])
            hT = sb(P, name + "hT", [128, 4, NT], BF16)
            xt = sb(P, name + "xt", [128, D]); xb2 = sb(P, name + "xb", [128, D], BF16)
            sil = sb(P, name + "sil", [128, 512])
            gB2 = sb(P, name + "gB", [128, D]); bB2 = sb(P, name + "bB", [128, D])
            load("sp", gB2, gB2[:], lng.partition_broadcast(128))
            load("sp", bB2, bB2[:], lnb.partition_broadcast(128))
            wset = [(sb(P, name + "w1_%d" % i, [128, 8, 512], BF16), sb(P, name + "w3_%d" % i, [128, 8, 512], BF16),
                     sb(P, name + "w2_%d" % i, [128, 4, D], BF16)) for i in range(2)]
            gate = None
            if router is not None:
                gate = sb(P, name + "gate", [128, NTL, NE])
                rw = sb(P, name + "rw", [128, 8, NE]); xTf = sb(P, name + "xTf", [128, 8, 128])
                lg = sb(P, name + "lg", [128, NE]); m8 = sb(P, name + "m8", [128, 8]); g12 = sb(P, name + "g12", [128, 4])
                ga = sb(P, name + "ga", [128, NE])
                S.dma("sp", lambda: SP.dma_start(out=rw[:], in_=router.rearrange("(c p) e -> p c e", p=128)), writes=[rw])
            for j, (src, n) in enumerate(src_rows):
                load("sp", xt, xt[:n, :], src)
                S.op("act", lambda j=j, n=n: ACT.mul(out=acc[:n, j, :], in_=xt[:n, :], mul=ALPHA), reads=[xt], writes=[acc])
                to_fm(xt, n, xb2, xT2, cols[j][0])
                if router is not None:
                    p1 = bank(); p2 = bank()
                    for k in range(8):
                        pb = p1 if k < 4 else p2
                        transpose(pb, pb[:, (k % 4) * 128:(k % 4) * 128 + n], xt[:n, k * 128:(k + 1) * 128], ident_f[:n, :n], [xt, ident_f])
                    S.op("dve", lambda n=n: DVE.tensor_copy(out=xTf[:, 0:4, :n], in_=p1[:, :].rearrange("p (k t) -> p k t", k=4)[:, :, :n]),
                         reads=[p1], writes=[xTf])
                    S.op("dve", lambda n=n: DVE.tensor_copy(out=xTf[:, 4:8, :n], in_=p2[:, :].rearrange("p (k t) -> p k t", k=4)[:, :, :n]),
                         reads=[p2], writes=[xTf])
                    pb = bank()
                    mm(pb, pb[:n, :NE], [(xTf[:, k, :n], rw[:, k, :]) for k in range(8)], [xTf, rw])
                    S.op("dve", lambda n=n, pb=pb: DVE.tensor_copy(out=lg[:n, :], in_=pb[:n, :NE]), reads=[pb], writes=[lg])
                    S.op("dve", lambda n=n: DVE.max(out=m8[:n, :], in_=lg[:n, :]), reads=[lg], writes=[m8])
                    S.op("dve", lambda n=n: DVE.tensor_tensor(out=g12[:n, 0:1], in0=m8[:n, 1:2], in1=m8[:n, 0:1], op=ALU.subtract),
                         reads=[m8], writes=[g12])
                    S.op("act", lambda n=n: ACT.activation(out=g12[:n, 1:2], in_=g12[:n, 0:1], func=AF.Exp), reads=[g12], writes=[g12])
                    S.op("dve", lambda n=n: DVE.tensor_scalar(out=g12[:n, 1:2], in0=g12[:n, 1:2], scalar1=1.0, scalar2=None, op0=ALU.add),
                         reads=[g12], writes=[g12])
                    S.op("dve", lambda n=n: DVE.reciprocal(out=g12[:n, 2:3], in_=g12[:n, 1:2]), reads=[g12], writes=[g12])
                    S.op("dve", lambda n=n: DVE.tensor_scalar(out=g12[:n, 3:4], in0=g12[:n, 2:3], scalar1=-1.0, scalar2=1.0,
                                                              op0=ALU.mult, op1=ALU.add), reads=[g12], writes=[g12])
                    S.op("dve", lambda n=n: DVE.tensor_scalar(out=ga[:n, :], in0=lg[:n, :], scalar1=m8[:n, 0:1], scalar2=g12[:n, 2:3],
                                                              op0=ALU.is_equal, op1=ALU.mult), reads=[lg, m8, g12], writes=[ga])
                    S.op("dve", lambda n=n, j=j: DVE.tensor_scalar(out=gate[:n, j, :], in0=lg[:n, :], scalar1=m8[:n, 1:2], scalar2=g12[:n, 3:4],
                                                                   op0=ALU.is_equal, op1=ALU.mult), reads=[lg, m8, g12], writes=[gate])
                    S.op("dve", lambda n=n, j=j: DVE.tensor_tensor(out=gate[:n, j, :], in0=gate[:n, j, :], in1=ga[:n, :], op=ALU.add),
                         reads=[gate, ga], writes=[gate])
            gi = 0
            for (w1a, w3a, w2a, F, ge) in experts:
                f0 = 0
                while f0 < F:
                    fw = min(512, F - f0)
                    nfc = fw // 128
                    w1g, w3g, w2g = wset[gi % 2]; gi += 1
                    load_w_bf(w1g, w1g[:, :, :fw], w1a[:, f0:f0 + fw].rearrange("(c p) n -> p c n", p=128))
                    load_w_bf(w3g, w3g[:, :, :fw], w3a[:, f0:f0 + fw].rearrange("(c p) n -> p c n", p=128))
                    load_w_bf(w2g, w2g[:, :nfc, :], w2a[f0:f0 + fw, :].rearrange("(c p) n -> p c n", p=128))
                    for fc in range(nfc):
                        for (cc, n) in ntiles:
                            p1 = bank(); p3 = bank()
                            mm(p1, p1[:, :n], [(w1g[:, k, fc * 128:(fc + 1) * 128], xT2[:, k, cc:cc + n]) for k in range(8)], [w1g, xT2])
                            mm(p3, p3[:, :n], [(w3g[:, k, fc * 128:(fc + 1) * 128], xT2[:, k, cc:cc + n]) for k in range(8)], [w3g, xT2])
                            S.op("act", lambda p1=p1, n=n: ACT.activation(out=sil[:, :n], in_=p1[:, :n], func=AF.Silu), reads=[p1], writes=[sil])
                            S.op("dve", lambda p3=p3, n=n, fc=fc, cc=cc: DVE.tensor_tensor(out=hT[:, fc, cc:cc + n], in0=p3[:, :n], in1=sil[:, :n],
                                                                                        op=ALU.mult), reads=[p3, sil], writes=[hT])
                    for j, (cc, n) in enumerate(cols):
                        for hf in range(2):
                            pb = bank()
                            mm(pb, pb[:n, :], [(hT[:, fc, cc:cc + n], w2g[:, fc, hf * 512:(hf + 1) * 512]) for fc in range(nfc)], [hT, w2g])
                            if ge is None:
                                S.op("dve", lambda pb=pb, n=n, j=j, hf=hf: DVE.tensor_tensor(
                                    out=acc[:n, j, hf * 512:(hf + 1) * 512], in0=pb[:n, :], in1=acc[:n, j, hf * 512:(hf + 1) * 512], op=ALU.add),
                                    reads=[pb, acc], writes=[acc])
                            else:
                                S.op("dve", lambda pb=pb, n=n, j=j, hf=hf, ge=ge: DVE.scalar_tensor_tensor(
                                    out=acc[:n, j, hf * 512:(hf + 1) * 512], in0=pb[:n, :], scalar=gate[:n, j, ge:ge + 1],
                                    in1=acc[:n, j, hf * 512:(hf + 1) * 512], op0=ALU.mult, op1=ALU.add), reads=[pb, acc, gate], writes=[acc])
                    f0 += fw
            for j, (dst, n) in enumerate(dst_rows):
                S.op("pool", lambda j=j, n=n: POOL.tensor_copy(out=xt[:n, :], in_=acc[:n, j, :]), reads=[acc], writes=[xt])
                layernorm_rows(lnst, xt, n, gB2, bB2, xt, name)
                store("sp", xt, dst, xt[:n, :])
        S.barrier()

    def rows(t, a, b):
        return [(t[r:r + 128, :], 128) for r in range(a, b, 128)]

    ffn = [(ffn_w1, ffn_w3, ffn_w2, DFF, None)]
    gated_pass("f0", rows(xm_scr, 0, HALF), ffn, ln_ffn_even_g, ln_ffn_even_b, rows(x1_scr, 0, HALF))
    gated_pass("f1", rows(xm_scr, HALF, SEQ) + [(xm_scr[SEQ:SEQ + NS, :], NS)], ffn, ln_ffn_even_g, ln_ffn_even_b,
               rows(x1_scr, HALF, SEQ) + [(x1_scr[SEQ:SEQ + NS, :], NS)])

    with ExitStack() as P3:
        KT = sb(P3, "KT", [128, 4, SEQ], BF16); QT = sb(P3, "QT", [128, 4, HALF], BF16)
        Vx = sb(P3, "Vx", [128, 32, 8, 65], BF16)
        yTd = sb(P3, "yTd", [128, 4, HALF], BF16)
        negFm = sb(P3, "negFm", [128, 32, 8]); Fend = sb(P3, "Fend", [128, 16, 8])
        pmk = sb(P3, "pmk", [128, 32]); hfl = sb(P3, "hfl", [128, 1]); one1 = sb(P3, "one1", [128, 1])
        fbB = sb(P3, "fbB", [128, 8]); tri = sb(P3, "tri", [128, 128]); sel127 = sb(P3, "sel127", [128, 128])
        maskT = sb(P3, "maskT", [128, 128], BF16); maskf = sb(P3, "maskf", [128, 128])
        carry = sb(P3, "carry", [128, 8]); cdw = sb(P3, "cdw", [128, 4, 3]); cdw_t = sb(P3, "cdw_t", [3, 512])
        load("sp", pmk, pmk[:], pmask); load("sp", hfl, hfl[:], hflag)
        load("sp", fbB, fbB[:], forget_bias.partition_broadcast(128))
        load("sp", cdw_t, cdw_t[:], conv_d_w)
        pb = bank()
        for c in range(4):
            transpose(pb, pb[:, c * 3:(c + 1) * 3], cdw_t[:, c * 128:(c + 1) * 128], ident_f[:3, :3], [cdw_t, ident_f])
        S.op("dve", lambda: DVE.tensor_copy(out=cdw[:], in_=pb[:, 0:12].rearrange("p (c j) -> p c j", c=4)), reads=[pb], writes=[cdw])
        S.op("pool", lambda: POOL.memset(one1[:], 1.0), writes=[one1])
        S.op("pool", lambda: POOL.memset(carry[:], 0.0), writes=[carry])
        S.op("pool", lambda: POOL.memset(Vx[:], 1.0), writes=[Vx])
        S.op("pool", lambda: POOL.affine_select(out=tri[:], in_=ones_f[:], pattern=[[1, 128]], compare_op=ALU.is_ge, fill=0.0,
                                                 base=0, channel_multiplier=-1), reads=[ones_f], writes=[tri])
        S.op("pool", lambda: POOL.affine_select(out=sel127[:], in_=ones_f[:], pattern=[[0, 128]], compare_op=ALU.is_equal, fill=0.0,
                                                 base=-127, channel_multiplier=1), reads=[ones_f], writes=[sel127])
        S.op("pool", lambda: POOL.memset(maskf[:], 0.0), writes=[maskf])
        S.op("pool", lambda: POOL.affine_select(out=maskf[:], in_=maskf[:], pattern=[[1, 128]], compare_op=ALU.is_ge, fill=NEG,
                                                 base=0, channel_multiplier=-1), reads=[maskf], writes=[maskf])
        S.op("pool", lambda: POOL.tensor_copy(out=maskT[:], in_=maskf[:]), reads=[maskf], writes=[maskT])

        qs_tok = sb(P3, "qs_tok", [NS, 512]); knew = sb(P3, "knew", [NS, 512]); vnew = sb(P3, "vnew", [NS, 512]); lfnew = sb(P3, "lfnew", [NS, 8])
        yTds = sb(P3, "yTds", [128, 4, NS], BF16); xs1 = sb(P3, "xs1", [NS, D])
        with ExitStack() as P3a:
            w_odd = sb(P3a, "w_odd", [128, 8, ODD_IN], BF16)
            load_w_bf(w_odd, w_odd[:], w_in_odd.rearrange("(c p) n -> p c n", p=128))
            x1t = sb(P3a, "x1t", [128, D]); x1b = sb(P3a, "x1b", [128, D], BF16); x1T = sb(P3a, "x1T", [128, 8, 512], BF16)
            ktok = sb(P3a, "ktok", [128, 512]); vtok = sb(P3a, "vtok", [128, 512]); lft = sb(P3a, "lft", [128, 8]); ft = sb(P3a, "ft", [128, 8])
            hf32 = sb(P3a, "hf32", [128, 512]); ud = sb(P3a, "ud", [128, 4, 514]); yd = sb(P3a, "yd", [128, 512]); bg = sb(P3a, "bg", [128, 512])
            S.op("pool", lambda: POOL.memset(ud[:], 0.0), writes=[ud])

            def logf_from(psb, n, dst):
                S.op("dve", lambda: DVE.tensor_tensor(out=dst[:n, :], in0=psb[:n, 0:8], in1=fbB[:n, :], op=ALU.add), reads=[psb, fbB], writes=[dst])
                S.op("act", lambda: ACT.activation(out=dst[:n, :], in_=dst[:n, :], func=AF.Exp, scale=-1.0), reads=[dst], writes=[dst])
                S.op("act", lambda: ACT.activation(out=dst[:n, :], in_=dst[:n, :], func=AF.Ln, bias=one1[:n, :], scale=1.0), reads=[dst, one1], writes=[dst])
                S.op("dve", lambda: DVE.tensor_scalar(out=dst[:n, :], in0=dst[:n, :], scalar1=-1.0, scalar2=None, op0=ALU.mult), reads=[dst], writes=[dst])

            for it in range(SEQ // 512):
                t0 = it * 512
                own = it >= 4
                for tt in range(4):
                    load("sp", x1t, x1t[:, :], x1_scr[t0 + tt * 128:t0 + (tt + 1) * 128, :])
                    to_fm(x1t, 128, x1b, x1T, tt * 128)
                for c in range(4):
                    pb = bank()
                    mm(pb, pb[:, :], [(w_odd[:, k, 512 + c * 128:512 + (c + 1) * 128], x1T[:, k, :]) for k in range(8)], [w_odd, x1T])
                    S.op("act", lambda c=c, pb=pb, t0=t0: ACT.copy(out=KT[:, c, t0:t0 + 512], in_=pb[:, :]), reads=[pb], writes=[KT])
                    if own:
                        pb = bank()
                        mm(pb, pb[:, :], [(w_odd[:, k, c * 128:(c + 1) * 128], x1T[:, k, :]) for k in range(8)], [w_odd, x1T])
                        S.op("act", lambda c=c, pb=pb, t0=t0: ACT.mul(out=QT[:, c, t0 - HALF:t0 - HALF + 512], in_=pb[:, :], mul=0.125),
                             reads=[pb], writes=[QT])
                for tt in range(4):
                    tl = it * 4 + tt
                    r0 = t0 + tt * 128
                    pk = bank(); pv = bank(); pf = bank()
                    xs_ = [x1T[:, k, tt * 128:(tt + 1) * 128] for k in range(8)]
                    mm(pv, pv[:, :], [(xs_[k], w_odd[:, k, 1024:1536]) for k in range(8)], [x1T, w_odd])
                    mm(pf, pf[:, 0:8], [(xs_[k], w_odd[:, k, 1536:1544]) for k in range(8)], [x1T, w_odd])
                    S.op("act", lambda pv=pv, tl=tl: ACT.copy(out=Vx[:, tl, :, 0:64], in_=pv[:, :].rearrange("p (h d) -> p h d", h=8)),
                         reads=[pv], writes=[Vx])
                    logf_from(pf, 128, lft)
                    if own:
                        mm(pk, pk[:, :], [(xs_[k], w_odd[:, k, 512:1024]) for k in range(8)], [x1T, w_odd])
                        S.op("dve", lambda pk=pk: DVE.tensor_copy(out=ktok[:, :], in_=pk[:, :]), reads=[pk], writes=[ktok])
                        S.op("dve", lambda pv=pv: DVE.tensor_copy(out=vtok[:, :], in_=pv[:, :]), reads=[pv], writes=[vtok])
                        store("sp", ktok, k_own[r0 - HALF:r0 - HALF + 128, :], ktok[:, :])
                        store("sp", vtok, v_own[r0 - HALF:r0 - HALF + 128, :], vtok[:, :])
                        store("sp", lft, lf_own[r0 - HALF:r0 - HALF + 128, :], lft[:, :])
                    p1 = bank(); p2 = bank()
                    mm(p1, p1[:, 0:8], [(tri[:], lft[:, :])], [tri, lft])
                    mm(p2, p2[:, 0:8], [(ones_f[:], lft[:, :])], [ones_f, lft])
                    S.op("dve", lambda p1=p1: DVE.tensor_tensor(out=ft[:, :], in0=p1[:, 0:8], in1=carry[:, :], op=ALU.add), reads=[p1, carry], writes=[ft])
                    S.op("dve", lambda p2=p2: DVE.tensor_tensor(out=carry[:, :], in0=p2[:, 0:8], in1=carry[:, :], op=ALU.add), reads=[p2, carry], writes=[carry])
                    S.op("dve", lambda tl=tl: DVE.tensor_scalar(out=negFm[:, tl, :], in0=ft[:, :], scalar1=-1.0, scalar2=pmk[:, tl:tl + 1],
                                                                 op0=ALU.mult, op1=ALU.add), reads=[ft, pmk], writes=[negFm])
                    if own:
                        p3 = bank()
                        mm(p3, p3[:, 0:8], [(sel127[:], ft[:, :])], [sel127, ft])
                        S.op("dve", lambda p3=p3, tl=tl: DVE.tensor_copy(out=Fend[:, tl - 16, :], in_=p3[:, 0:8]), reads=[p3], writes=[Fend])
                if it >= 3:
                    for c in range(4):
                        ph = bank(); pc = bank()
                        mm(ph, ph[:, :], [(w_odd[:, k, 1544 + c * 128:1544 + (c + 1) * 128], x1T[:, k, :]) for k in range(8)], [w_odd, x1T])
                        mm(pc, pc[:, :], [(w_odd[:, k, 2568 + c * 128:2568 + (c + 1) * 128], x1T[:, k, :]) for k in range(8)], [w_odd, x1T])
                        S.op("act", lambda ph=ph: ACT.copy(out=hf32[:, :], in_=ph[:, :]), reads=[ph], writes=[hf32])
                        S.op("dve", lambda pc=pc, c=c: DVE.tensor_tensor(out=ud[:, c, 2:514], in0=pc[:, :], in1=hf32[:, :], op=ALU.mult),
                             reads=[pc, hf32], writes=[ud])
                        if own:
                            pg = bank()
                            mm(pg, pg[:, :], [(w_odd[:, k, 2056 + c * 128:2056 + (c + 1) * 128], x1T[:, k, :]) for k in range(8)], [w_odd, x1T])
                            S.op("act", lambda pg=pg: ACT.copy(out=bg[:, :], in_=pg[:, :]), reads=[pg], writes=[bg])
                            S.op("dve", lambda c=c: DVE.tensor_scalar(out=yd[:, :], in0=ud[:, c, 0:512], scalar1=cdw[:, c, 0:1], scalar2=None, op0=ALU.mult),
                                 reads=[ud, cdw], writes=[yd])
                            for j in (1, 2):
                                S.op("dve", lambda c=c, j=j: DVE.scalar_tensor_tensor(out=yd[:, :], in0=ud[:, c, j:j + 512], scalar=cdw[:, c, j:j + 1],
                                                                                     in1=yd[:, :], op0=ALU.mult, op1=ALU.add), reads=[ud, cdw, yd], writes=[yd])
                            S.op("dve", lambda c=c, t0=t0: DVE.tensor_tensor(out=yTd[:, c, t0 - HALF:t0 - HALF + 512], in0=yd[:, :], in1=bg[:, :], op=ALU.mult),
                                 reads=[yd, bg], writes=[yTd])
                    S.op("pool", lambda: POOL.tensor_copy(out=ud[:, :, 0:2], in_=ud[:, :, 512:514]), reads=[ud], writes=[ud])
                    if it == 3:
                        S.op("dve", lambda: DVE.tensor_scalar(out=ud[:, :, 0:2], in0=ud[:, :, 0:2], scalar1=hfl[:, 0:1], scalar2=None, op0=ALU.mult),
                             reads=[ud, hfl], writes=[ud])
            tl2 = sb(P3a, "tl2", [2, 512])
            pb = bank()
            for c in range(4):
                transpose(pb, pb[:2, c * 128:(c + 1) * 128], ud[:, c, 0:2], ident_f[:], [ud, ident_f])
            S.op("dve", lambda: DVE.tensor_copy(out=tl2[:, :], in_=pb[:2, :]), reads=[pb], writes=[tl2])
            store("sp", tl2, convd_tail, tl2[:, :])

            xs1T = sb(P3a, "xs1T", [128, 8, NS], BF16)
            load("sp", xs1, xs1[:, :], x1_scr[SEQ:SEQ + NS, :])
            to_fm(xs1, NS, x1b, xs1T, 0)
            for (off, dst, sc) in ((0, qs_tok, 0.125), (512, knew, 1.0), (1024, vnew, 1.0)):
                pb = bank()
                mm(pb, pb[:NS, :], [(xs1T[:, k, :], w_odd[:, k, off:off + 512]) for k in range(8)], [xs1T, w_odd])
                S.op("act", lambda pb=pb, dst=dst, sc=sc: ACT.mul(out=dst[:, :], in_=pb[:NS, :], mul=sc), reads=[pb], writes=[dst])
            pb = bank()
            mm(pb, pb[:NS, 0:8], [(xs1T[:, k, :], w_odd[:, k, 1536:1544]) for k in range(8)], [xs1T, w_odd])
            logf_from(pb, NS, lfnew)
            store("sp", knew, k_s, knew[:, :]); store("sp", vnew, v_s, vnew[:, :]); store("sp", lfnew, lf_s, lfnew[:, :])
            sd_tok = sb(P3a, "sd_tok", [NS * 2, 512]); uds = sb(P3a, "uds", [128, 4, NS, 3]); hs = sb(P3a, "hs", [128, NS]); bgs = sb(P3a, "bgs", [128, NS])
            prd = sb(P3a, "prd", [128, NS, 3]); yds = sb(P3a, "yds", [128, NS])
            load("sp", sd_tok, sd_tok[:, :], sconvd)
            pb = bank()
            for c in range(4):
                transpose(pb, pb[:, c * 8:(c + 1) * 8], sd_tok[:, c * 128:(c + 1) * 128], ident_f[:8, :8], [sd_tok, ident_f])
            S.op("dve", lambda: DVE.tensor_copy(out=uds[:, :, :, 0:2], in_=pb[:, 0:32].rearrange("p (c b j) -> p c b j", c=4, b=NS)),
                 reads=[pb], writes=[uds])
            for c in range(4):
                ph = bank(); pc = bank(); pg = bank()
                mm(ph, ph[:, :NS], [(w_odd[:, k, 1544 + c * 128:1544 + (c + 1) * 128], xs1T[:, k, :]) for k in range(8)], [w_odd, xs1T])
                mm(pc, pc[:, :NS], [(w_odd[:, k, 2568 + c * 128:2568 + (c + 1) * 128], xs1T[:, k, :]) for k in range(8)], [w_odd, xs1T])
                mm(pg, pg[:, :NS], [(w_odd[:, k, 2056 + c * 128:2056 + (c + 1) * 128], xs1T[:, k, :]) for k in range(8)], [w_odd, xs1T])
                S.op("act", lambda ph=ph: ACT.copy(out=hs[:, :], in_=ph[:, :NS]), reads=[ph], writes=[hs])
                S.op("act", lambda pg=pg: ACT.copy(out=bgs[:, :], in_=pg[:, :NS]), reads=[pg], writes=[bgs])
                S.op("dve", lambda pc=pc, c=c: DVE.tensor_tensor(out=uds[:, c, :, 2], in0=pc[:, :NS], in1=hs[:, :], op=ALU.mult), reads=[pc, hs], writes=[uds])
                S.op("dve", lambda c=c: DVE.tensor_tensor(out=prd[:, :, :], in0=uds[:, c, :, :], in1=cdw[:, c:c + 1, :].to_broadcast([128, NS, 3]), op=ALU.mult),
                     reads=[uds, cdw], writes=[prd])
                S.op("dve", lambda: DVE.tensor_reduce(out=yds[:, :], in_=prd[:, :, :], axis=AX.X, op=ALU.add), reads=[prd], writes=[yds])
                S.op("dve", lambda c=c: DVE.tensor_tensor(out=yTds[:, c, :], in0=yds[:, :], in1=bgs[:, :], op=ALU.mult), reads=[yds, bgs], writes=[yTds])
            for b_ in range(NS):
                pb = bank()
                for c in range(4):
                    transpose(pb, pb[:2, c * 128:(c + 1) * 128], uds[:, c, b_, 1:3], ident_f[:], [uds, ident_f])
                S.op("dve", lambda pb=pb: DVE.tensor_copy(out=tl2[:, :], in_=pb[:2, :]), reads=[pb], writes=[tl2])
                store("sp", tl2, convd_s[b_, :, :], tl2[:, :])
        S.barrier()

        with ExitStack() as P4:
            w_oo = sb(P4, "w_oo", [128, 8, D], BF16)
            load_w_bf(w_oo, w_oo[:], w_out_odd.rearrange("(c p) n -> p c n", p=128))
            gB3 = sb(P4, "gB3", [128, D]); bB3 = sb(P4, "bB3", [128, D])
            load("sp", gB3, gB3[:], ln_mix_odd_g.partition_broadcast(128)); load("sp", bB3, bB3[:], ln_mix_odd_b.partition_broadcast(128))
            bias_ij = sb(P4, "bias_ij", [128, 8]); PT = sb(P4, "PT", [128, 128], BF16)
            otok = sb(P4, "otok", [128, 512]); rden = sb(P4, "rden", [128, 8]); ob = sb(P4, "ob", [128, D], BF16)
            oT = sb(P4, "oT", [128, 8, 128], BF16); x1r = sb(P4, "x1r", [128, D]); r4 = sb(P4, "r4", [128, D]); xo4 = sb(P4, "xo4", [128, D])
            pacc = [pbank[5], pbank[6]]
            srot = [pbank[i] for i in range(5)]
            si = [0]

            def outproj_ln(n, lhs_chunks, reads_, xres, dst):
                p1 = srot[si[0] % 5]; p2 = srot[(si[0] + 1) % 5]; si[0] += 2
                for hf, pb in ((0, p1), (1, p2)):
                    mm(pb, pb[:n, :], [(lhs_chunks[k], w_oo[:, k, hf * 512:(hf + 1) * 512]) for k in range(8)], reads_ + [w_oo])
                    S.op("dve", lambda hf=hf, pb=pb: DVE.scalar_tensor_tensor(out=r4[:n, hf * 512:(hf + 1) * 512], in0=xres[:n, hf * 512:(hf + 1) * 512],
                                                                              scalar=ALPHA, in1=pb[:n, :], op0=ALU.mult, op1=ALU.add),
                         reads=[xres, pb], writes=[r4])
                layernorm_rows(lnst, r4, n, gB3, bB3, xo4, "p4")
                store("sp", xo4, dst, xo4[:n, :])

            for i in range(16):
                for j in range(17 + i):
                    S.op("dve", lambda i=i, j=j: DVE.tensor_tensor(out=bias_ij[:, :], in0=negFm[:, j, :], in1=Fend[:, i, :], op=ALU.add),
                         reads=[negFm, Fend], writes=[bias_ij])
                    for h in range(8):
                        c, pbase = h // 2, (h % 2) * 64
                        pb = srot[si[0] % 5]; si[0] += 1
                        pairs = [(KT[pbase:pbase + 64, c, j * 128:(j + 1) * 128], QT[pbase:pbase + 64, c, i * 128:(i + 1) * 128])]
                        if j == 16 + i:
                            pairs.append((ident_b[:], maskT[:]))
                        mm(pb, pb[:, 0:128], pairs, [KT, QT, ident_b, maskT])
                        S.op("act", lambda pb=pb, h=h: ACT.activation(out=PT[:, :], in_=pb[:, 0:128], func=AF.Exp, bias=bias_ij[:, h:h + 1], scale=1.0),
                             reads=[pb, bias_ij], writes=[PT])
                        pa = pacc[h // 4]
                        S.op("pe", lambda pa=pa, h=h, j=j, i=i: PE.matmul(pa[:, (h % 4) * 65:(h % 4) * 65 + 65], lhsT=PT[:, :], rhs=Vx[:, j, h, :],
                                                                        start=(j == 0), stop=(j == 16 + i)), reads=[PT, Vx], writes=[pa])
                for hh in range(2):
                    pa = pacc[hh]
                    S.op("dve", lambda pa=pa, hh=hh: DVE.reciprocal(out=rden[:, hh * 4:(hh + 1) * 4], in_=pa[:, 0:260].rearrange("p (h d) -> p h d", h=4)[:, :, 64]),
                         reads=[pa], writes=[rden])
                    S.op("dve", lambda pa=pa, hh=hh: DVE.tensor_tensor(
                        out=otok[:, hh * 256:(hh + 1) * 256].rearrange("p (h d) -> p h d", h=4),
                        in0=pa[:, 0:260].rearrange("p (h d) -> p h d", h=4)[:, :, 0:64],
                        in1=rden[:, hh * 4:(hh + 1) * 4].unsqueeze(2).to_broadcast([128, 4, 64]), op=ALU.mult), reads=[pa, rden], writes=[otok])
                S.op("pool", lambda: POOL.tensor_copy(out=ob[:, 0:512], in_=otok[:, :]), reads=[otok], writes=[ob])
                for k in range(4):
                    transpose(ptr, ptr[:, k * 128:(k + 1) * 128], ob[:, k * 128:(k + 1) * 128], ident_b[:], [ob, ident_b])
                S.op("act", lambda: ACT.copy(out=oT[:, 0:4, :], in_=ptr[:, 0:512].rearrange("p (k t) -> p k t", k=4)), reads=[ptr], writes=[oT])
                load("sp", x1r, x1r[:, :], x1_scr[HALF + i * 128:HALF + (i + 1) * 128, :])
                outproj_ln(128, [oT[:, k, :] for k in range(4)] + [yTd[:, c, i * 128:(i + 1) * 128] for c in range(4)], [oT, yTd], x1r,
                           x2_scr[i * 128:(i + 1) * 128, :])

            pts = sb(P4, "pts", [128, NS * NPAGES], I32); idx = sb(P4, "idx", [128, NS * NPAGES], I32); iot = sb(P4, "iot", [128, 1], I32)
            S.dma("pool", lambda: POOL.dma_start(out=pts[:], in_=ptab.rearrange("b p -> (b p)").partition_broadcast(128)), writes=[pts])
            S.op("pool", lambda: POOL.iota(iot[:], [[0, 1]], base=0, channel_multiplier=1), writes=[iot])
            S.op("pool", lambda: POOL.tensor_scalar(out=idx[:], in0=pts[:], scalar1=128, scalar2=None, op0=ALU.mult), reads=[pts], writes=[idx])
            S.op("pool", lambda: POOL.tensor_tensor(out=idx[:], in0=idx[:], in1=iot[:].to_broadcast([128, NS * NPAGES]), op=ALU.add),
                 reads=[idx, iot], writes=[idx])
            NP1 = NPAGES + 1
            selb = sb(P4, "selb", [NS, NS, 128]); qb = sb(P4, "qb", [128, 512])
            kpg = sb(P4, "kpg", [128, 512]); vpg = sb(P4, "vpg", [128, 512]); prod2 = sb(P4, "prod2", [128, 512])
            Ssc = sb(P4, "Ssc", [128, NP1, 8]); lfp = sb(P4, "lfp", [128, NP1, 8]); Gs = sb(P4, "Gs", [128, NP1, 8]); Gt = sb(P4, "Gt", [128, NP1, 8])
            Ee = sb(P4, "Ee", [128, NP1, 8]); esum = sb(P4, "esum", [128, 8]); rd8 = sb(P4, "rd8", [8, 1]); on8 = sb(P4, "on8", [8, 512])
            bdm = sb(P4, "bdm", [8, 512]); oTs = sb(P4, "oTs", [128, 4, NS], BF16); sut = sb(P4, "sut", [128, 128])
            S.op("pool", lambda: POOL.memset(selb[:], 1.0), writes=[selb])
            S.op("pool", lambda: POOL.affine_select(out=selb[:], in_=selb[:], pattern=[[1, NS], [0, 128]], compare_op=ALU.is_equal, fill=0.0,
                                                     base=0, channel_multiplier=-1), reads=[selb], writes=[selb])
            S.op("pool", lambda: POOL.memset(bdm[:], 1.0), writes=[bdm])
            S.op("pool", lambda: POOL.affine_select(out=bdm[:], in_=bdm[:], pattern=[[1, 8], [0, 64]], compare_op=ALU.is_equal, fill=0.0,
                                                     base=0, channel_multiplier=-1), reads=[bdm], writes=[bdm])
            S.op("pool", lambda: POOL.affine_select(out=sut[:], in_=ones_f[:], pattern=[[-1, 128]], compare_op=ALU.is_gt, fill=0.0,
                                                     base=0, channel_multiplier=1), reads=[ones_f], writes=[sut])
            for b_ in range(NS):
                pb = srot[si[0] % 5]; si[0] += 1
                mm(pb, pb[:, :], [(selb[:, b_, :], qs_tok[:, :])], [selb, qs_tok])
                S.op("act", lambda pb=pb: ACT.copy(out=qb[:, :], in_=pb[:, :]), reads=[pb], writes=[qb])
                S.op("pool", lambda: POOL.memset(lfp[:, NPAGES, :], 0.0), writes=[lfp])
                for p in range(NPAGES):
                    S.dma("pool", lambda p=p: POOL.indirect_dma_start(out=lfp[:, p, :], out_offset=None, in_=cache_lf,
                                                                       in_offset=bass.IndirectOffsetOnAxis(ap=idx[:, b_ * NPAGES + p:b_ * NPAGES + p + 1], axis=0)),
                          reads=[idx], writes=[lfp])
                S.dma("sp", lambda: SP.dma_start(out=lfp[0:1, NPAGES, :], in_=lfnew[b_:b_ + 1, :]), reads=[lfnew], writes=[lfp])
                NPG = NPAGES
                p1 = srot[si[0] % 5]; p2 = srot[(si[0] + 1) % 5]; p3_ = srot[(si[0] + 2) % 5]; si[0] += 3
                lf2 = lfp[:, 0:NPG, :].rearrange("p a h -> p (a h)")
                mm(p1, p1[:, 0:NPG * 8], [(sut[:], lf2)], [sut, lfp])
                mm(p2, p2[:, 0:NPG * 8], [(ones_f[:], lf2)], [ones_f, lfp])
                mm(p3_, p3_[:, 0:8], [(selb[:, b_, :], lfnew[:, :])], [selb, lfnew])
                S.op("dve", lambda p2=p2: DVE.tensor_copy(out=Gt[:, 0:NPG, :], in_=p2[:, 0:NPG * 8].rearrange("p (a h) -> p a h", h=8)), reads=[p2], writes=[Gt])
                S.op("dve", lambda: DVE.tensor_copy(out=Ee[:, 0:NPG, :], in_=Gt[:, 0:NPG, :]), reads=[Gt], writes=[Ee])
                src, dst = Gt, Gs
                sft = 1
                while sft < NPG:
                    S.op("dve", lambda src=src, dst=dst, sft=sft: DVE.tensor_tensor(out=dst[:, 0:NPG - sft, :], in0=src[:, 0:NPG - sft, :],
                                                                                 in1=src[:, sft:NPG, :], op=ALU.add), reads=[src], writes=[dst])
                    S.op("dve", lambda src=src, dst=dst, sft=sft: DVE.tensor_copy(out=dst[:, NPG - sft:NPG, :], in_=src[:, NPG - sft:NPG, :]),
                         reads=[src], writes=[dst])
                    src, dst = dst, src
                    sft *= 2
                incl = src
                other = dst
                S.op("dve", lambda: DVE.tensor_tensor(out=other[:, 0:NPG, :], in0=incl[:, 0:NPG, :], in1=Ee[:, 0:NPG, :], op=ALU.subtract), reads=[incl, Ee], writes=[other])
                S.op("dve", lambda p1=p1: DVE.tensor_tensor(out=other[:, 0:NPG, :], in0=other[:, 0:NPG, :], in1=p1[:, 0:NPG * 8].rearrange("p (a h) -> p a h", h=8),
                                                            op=ALU.add), reads=[other, p1], writes=[other])
                S.op("dve", lambda p3_=p3_: DVE.tensor_copy(out=esum[:, :], in_=p3_[:, 0:8]), reads=[p3_], writes=[esum])
                S.op("dve", lambda: DVE.tensor_tensor(out=other[:, 0:NPG, :], in0=other[:, 0:NPG, :], in1=esum[:, :].unsqueeze(1).to_broadcast([128, NPG, 8]),
                                                      op=ALU.add), reads=[other, esum], writes=[other])
                Gfin = other
                S.op("pool", lambda: POOL.memset(Gfin[:, NPAGES, :], 0.0), writes=[Gfin])
                S.op("pool", lambda: POOL.affine_select(
                    out=Gfin[:, NPAGES, :], in_=Gfin[:, NPAGES, :], pattern=[[0, 8]], compare_op=ALU.is_equal, fill=NEG, base=0, channel_multiplier=1),
                    reads=[Gfin], writes=[Gfin])
                for p in range(NP1):
                    if p < NPAGES:
                        S.dma("pool", lambda p=p: POOL.indirect_dma_start(out=kpg[:, :], out_offset=None, in_=cache_k,
                                                                           in_offset=bass.IndirectOffsetOnAxis(ap=idx[:, b_ * NPAGES + p:b_ * NPAGES + p + 1], axis=0)),
                              reads=[idx], writes=[kpg])
                    else:
                        S.op("pool", lambda: POOL.memset(kpg[:, :], 0.0), writes=[kpg])
                        S.dma("sp", lambda: SP.dma_start(out=kpg[0:1, :], in_=knew[b_:b_ + 1, :]), reads=[knew], writes=[kpg])
                    S.op("dve", lambda: DVE.tensor_tensor(out=prod2[:, :], in0=kpg[:, :], in1=qb[:, :], op=ALU.mult), reads=[kpg, qb], writes=[prod2])
                    S.op("dve", lambda p=p: DVE.tensor_reduce(out=Ssc[:, p, :], in_=prod2[:, :].rearrange("p (h d) -> p h d", h=8), axis=AX.X, op=ALU.add),
                         reads=[prod2], writes=[Ssc])
                S.op("dve", lambda: DVE.tensor_tensor(out=Ssc[:, :, :], in0=Ssc[:, :, :], in1=Gfin[:, :, :], op=ALU.add), reads=[Ssc, Gfin], writes=[Ssc])
                S.op("act", lambda: ACT.activation(out=Ee[:, :, :], in_=Ssc[:, :, :], func=AF.Exp), reads=[Ssc], writes=[Ee])
                S.op("dve", lambda: DVE.tensor_reduce(out=esum[:, :], in_=Ee[:, :, :].rearrange("p a h -> p h a"), axis=AX.X, op=ALU.add), reads=[Ee], writes=[esum])
                pa = pacc[0]
                for p in range(NP1):
                    if p < NPAGES:
                        S.dma("pool", lambda p=p: POOL.indirect_dma_start(out=vpg[:, :], out_offset=None, in_=cache_v,
                                                                           in_offset=bass.IndirectOffsetOnAxis(ap=idx[:, b_ * NPAGES + p:b_ * NPAGES + p + 1], axis=0)),
                              reads=[idx], writes=[vpg])
                    else:
                        S.op("pool", lambda: POOL.memset(vpg[:, :], 0.0), writes=[vpg])
                        S.dma("sp", lambda: SP.dma_start(out=vpg[0:1, :], in_=vnew[b_:b_ + 1, :]), reads=[vnew], writes=[vpg])
                    S.op("pe", lambda p=p, pa=pa: PE.matmul(pa[:8, :], lhsT=Ee[:, p, :], rhs=vpg[:, :], start=(p == 0), stop=(p == NP1 - 1)),
                         reads=[Ee, vpg], writes=[pa])
                pd = pacc[1]
                mm(pd, pd[:8, 0:1], [(esum[:, :], ones_f[:, 0:1])], [esum, ones_f])
                S.op("dve", lambda pd=pd: DVE.reciprocal(out=rd8[:, :], in_=pd[:8, 0:1]), reads=[pd], writes=[rd8])
                S.op("dve", lambda pa=pa: DVE.scalar_tensor_tensor(out=on8[:, :], in0=pa[:8, :], scalar=rd8[:, 0:1], in1=bdm[:, :], op0=ALU.mult, op1=ALU.mult),
                     reads=[pa, rd8, bdm], writes=[on8])
                pb = srot[si[0] % 5]; si[0] += 1
                for c in range(4):
                    mm(pb, pb[:, c:c + 1], [(on8[:, c * 128:(c + 1) * 128], ones_f[:8, 0:1])], [on8, ones_f])
                S.op("act", lambda pb=pb: ACT.copy(out=oTs[:, :, b_], in_=pb[:, 0:4]), reads=[pb], writes=[oTs])
            outproj_ln(NS, [oTs[:, k, :] for k in range(4)] + [yTds[:, c, :] for c in range(4)], [oTs, yTds], xs1, x2_scr[HALF:HALF + NS, :])
        S.barrier()
    S.barrier()

    moe = [(moe_w1[e], moe_w3[e], moe_w2[e], DFE, e) for e in range(NE)]
    gated_pass("mo", rows(x2_scr, 0, HALF) + [(x2_scr[HALF:HALF + NS, :], NS)], moe, ln_ffn_odd_g, ln_ffn_odd_b,
               rows(y_own, 0, HALF) + [(y_s[:, :], NS)], router=router_w)
    S.barrier()
    return


POOL_WINDOWS = (2, 4, 8, 16)
W_NAMES = ["w_in_even", "pool_w", "pool_scale", "conv_b_w", "conv_b_bias", "conv_ln_g", "conv_ln_b", "w_out_even",
           "ln_mix_even_g", "ln_mix_even_b", "ffn_w1", "ffn_w3", "ffn_w2", "ln_ffn_even_g", "ln_ffn_even_b",
           "w_in_odd", "forget_bias", "conv_d_w", "w_out_odd", "ln_mix_odd_g", "ln_mix_odd_b", "router_w",
           "moe_w1", "moe_w3", "moe_w2", "ln_ffn_odd_g", "ln_ffn_odd_b"]


def make_core_inputs(inp, c, compact_cache=False):
    b, h = c // 2, c % 2
    f = np.float32
    xp = np.asarray(inp["x_prompt"])
    m = {}
    if h == 1:
        m["xloc"] = np.ascontiguousarray(xp[b])
    else:
        m["xloc"] = np.concatenate([np.zeros((HALF, D), f), xp[b, :HALF]], axis=0)
    sl = slice(NS * c, NS * c + NS)
    m["xs"] = np.ascontiguousarray(np.asarray(inp["x_sample"])[sl, 0])
    m["spool"] = np.ascontiguousarray(np.asarray(inp["state_pool"])[0, sl]).reshape(NS * 15, 512)
    m["sconvb"] = np.ascontiguousarray(np.asarray(inp["state_conv_b"])[0, sl]).reshape(NS * 30, 512)
    m["sconvd"] = np.ascontiguousarray(np.asarray(inp["state_conv_d"])[0, sl]).reshape(NS * 2, 512)
    pt = np.asarray(inp["page_table"])[sl].astype(np.int32)
    ck, cv, cl = (np.asarray(inp[k])[0] for k in ("cache_k", "cache_v", "cache_logf"))
    if compact_cache:
        ids = pt.reshape(-1)
        ck, cv, cl = ck[ids], cv[ids], cl[ids]
        pt = np.arange(ids.size, dtype=np.int32).reshape(pt.shape)
    m["cache_k"] = ck.reshape(-1, 512); m["cache_v"] = cv.reshape(-1, 512); m["cache_lf"] = cl.reshape(-1, 8)
    m["ptab"] = np.ascontiguousarray(pt)
    pm = np.zeros((128, 32), f)
    if h == 0:
        pm[:, :16] = NEG
    m["pmask"] = pm
    gpos = np.arange(SEQ) if h == 1 else np.concatenate([np.full(HALF, 100000), np.arange(HALF)])
    m["invcnt"] = np.stack([1.0 / np.minimum(gpos + 1, w) for w in POOL_WINDOWS]).astype(f)
    m["hflag"] = np.full((128, 1), float(h), f)
    for k in W_NAMES:
        a = np.asarray(inp[k])
        m[k] = np.ascontiguousarray(a[0]) if a.shape[0] == 1 else a
    return m


def assemble(results, ncores=8):
    f = np.float32
    nb = ncores // 2
    y = np.zeros((nb, SEQ, D), f); ys = np.zeros((ncores * NS, 1, D), f)
    pool_p = np.zeros((1, nb, 15, 512), f); pool_s = np.zeros((1, ncores * NS, 15, 512), f)
    convb_p = np.zeros((1, nb, 30, 512), f); convb_s = np.zeros((1, ncores * NS, 30, 512), f)
    k_p = np.zeros((1, nb, SEQ, 8, 64), f); k_s = np.zeros((1, ncores * NS, 1, 8, 64), f)
    v_p = np.zeros((1, nb, SEQ, 8, 64), f); v_s = np.zeros((1, ncores * NS, 1, 8, 64), f)
    lf_p = np.zeros((1, nb, SEQ, 8), f); lf_s = np.zeros((1, ncores * NS, 1, 8), f)
    cd_p = np.zeros((1, nb, 2, 512), f); cd_s = np.zeros((1, ncores * NS, 2, 512), f)
    for c, r in enumerate(results):
        b, h = c // 2, c % 2
        ts = slice(h * HALF, (h + 1) * HALF); ss = slice(c * NS, (c + 1) * NS)
        y[b, ts] = r["y_own"]; ys[ss, 0] = r["y_s"]
        k_p[0, b, ts] = np.asarray(r["k_own"]).reshape(HALF, 8, 64); v_p[0, b, ts] = np.asarray(r["v_own"]).reshape(HALF, 8, 64)
        lf_p[0, b, ts] = r["lf_own"]
        k_s[0, ss, 0] = np.asarray(r["k_s"]).reshape(NS, 8, 64); v_s[0, ss, 0] = np.asarray(r["v_s"]).reshape(NS, 8, 64)
        lf_s[0, ss, 0] = r["lf_s"]
        pool_s[0, ss] = r["pool_s"]; convb_s[0, ss] = r["convb_s"]; cd_s[0, ss] = r["convd_s"]
        if h == 1:
            pool_p[0, b] = r["pool_tail"]; convb_p[0, b] = r["convb_tail"]; cd_p[0, b] = r["convd_tail"]
    return (y, ys, pool_p, pool_s, convb_p, convb_s, k_p, k_s, v_p, v_s, lf_p, lf_s, cd_p, cd_s)


def kernel(**inputs):
    nc = build(n_pool=2560, dev=False)
    maps = [make_core_inputs(inputs, c) for c in range(8)]
    res = run_bass_kernel_spmd(nc, maps, core_ids=list(range(8)))
    return assemble(res.results, 8)
```

```python
import numpy as np
from contextlib import ExitStack
import concourse.bass as bass
import concourse.mybir as mybir
from concourse.bass_utils import run_bass_kernel_spmd

F32 = mybir.dt.float32
BF16 = mybir.dt.bfloat16
I32 = mybir.dt.int32
ALU = mybir.AluOpType
AF = mybir.ActivationFunctionType
AX = mybir.AxisListType

D = 1024
SEQ = 4096
HALF = 2048
NS = 4
DFF = 2816
NE = 8
DFE = 3584
ODD_IN = 3080
ALPHA = float(4 ** 0.25)
EPS = 1e-5
NEG = -30000.0
NPAGES = 64


class Buf:
    __slots__ = ("t", "w", "r")

    def __init__(self, t):
        self.t = t
        self.w = None
        self.r = {}

    def __getitem__(self, k):
        return self.t[k]


class Sync:
    LIMIT = 30000

    def __init__(self, nc, es):
        self.nc = nc
        self.es = es
        self.engs = {"pe": nc.tensor, "act": nc.scalar, "dve": nc.vector, "pool": nc.gpsimd, "sp": nc.sync}
        self.sems = []
        self.cur = {}
        self.cnt = {}
        self.seen = {e: {} for e in self.engs}
        for e in self.engs:
            self._new_sem(e)
        self.dsem = []
        self.dval = []
        for i in range(24):
            self.sems.append(es.enter_context(nc.semaphore("d%d" % i)))
            self.dsem.append(len(self.sems) - 1)
            self.dval.append(0)
        self.dnext = 0
        self.last = {}

    def _new_sem(self, e):
        self.sems.append(self.es.enter_context(self.nc.semaphore("s%s%d" % (e, len(self.sems)))))
        self.cur[e] = len(self.sems) - 1
        self.cnt[e] = 0

    def wait(self, e, tk):
        if tk is None:
            return
        k, v = tk
        if self.seen[e].get(k, 0) >= v:
            return
        if k == self.cur.get(e) and e == "pe":
            return
        self.engs[e].wait_ge(self.sems[k], v)
        self.seen[e][k] = v

    def _deps(self, e, reads, writes):
        for b in reads:
            self.wait(e, b.w)
        for b in writes:
            self.wait(e, b.w)
            for t in b.r.values():
                self.wait(e, t)

    def _post(self, key, tk, reads, writes):
        for b in reads:
            b.r[key] = tk
        for b in writes:
            b.w = tk
            b.r = {}

    def op(self, e, fn, reads=(), writes=()):
        self._deps(e, reads, writes)
        if self.cnt[e] >= self.LIMIT:
            self._new_sem(e)
        ins = fn()
        self.cnt[e] += 1
        ins.then_inc(self.sems[self.cur[e]], 1)
        tk = (self.cur[e], self.cnt[e])
        self.last[e] = tk
        self._post(e, tk, reads, writes)
        return tk

    def dma(self, e, fn, reads=(), writes=()):
        self._deps(e, reads, writes)
        i = self.dnext
        self.dnext = (self.dnext + 1) % len(self.dsem)
        k = self.dsem[i]
        if self.dval[i] >= self.LIMIT:
            self.wait(e, (k, self.dval[i]))
            self.sems.append(self.es.enter_context(self.nc.semaphore("dd%d" % len(self.sems))))
            self.dsem[i] = len(self.sems) - 1
            self.dval[i] = 0
            k = self.dsem[i]
        if self.dval[i] > 0:
            self.wait(e, (k, self.dval[i]))
        ins = fn()
        self.dval[i] += 16
        ins.then_inc(self.sems[k], 16)
        tk = (k, self.dval[i])
        self.last[("d", i)] = tk
        self._post(("d", i), tk, reads, writes)
        return tk

    def barrier(self):
        tks = list(self.last.values())
        for e in self.engs:
            for tk in tks:
                self.wait(e, tk)


def build(n_pool=2560, dev=False):
    nc = bass.Bass("TRN2", target_bir_lowering=False)
    es = ExitStack()
    with es:
        _build(nc, es, n_pool, dev)
    return nc


def _build(nc, es, n_pool, dev):
    S = Sync(nc, es)
    PE, ACT, DVE, POOL, SP = nc.tensor, nc.scalar, nc.vector, nc.gpsimd, nc.sync

    def din(name, shape, dt=F32):
        return nc.dram_tensor(name, list(shape), dt, kind="ExternalInput").ap()

    def dout(name, shape, dt=F32):
        return nc.dram_tensor(name, list(shape), dt, kind="ExternalOutput").ap()

    def dscr(name, shape, dt=F32):
        return nc.dram_tensor(name, list(shape), dt, kind=("ExternalOutput" if dev else "Internal")).ap()

    def sb(st, name, shape, dt=F32):
        return Buf(st.enter_context(nc.sbuf_tensor(name, list(shape), dt)))

    def ps(st, name, shape, dt=F32):
        return Buf(st.enter_context(nc.psum_tensor(name, list(shape), dt)))

    xloc = din("xloc", [SEQ, D]); xs = din("xs", [NS, D])
    spool = din("spool", [NS * 15, 512]); sconvb = din("sconvb", [NS * 30, 512]); sconvd = din("sconvd", [NS * 2, 512])
    cache_k = din("cache_k", [n_pool * 128, 512]); cache_v = din("cache_v", [n_pool * 128, 512])
    cache_lf = din("cache_lf", [n_pool * 128, 8]); ptab = din("ptab", [NS, NPAGES], I32)
    pmask = din("pmask", [128, 32]); invcnt = din("invcnt", [4, SEQ]); hflag = din("hflag", [128, 1])
    w_in_even = din("w_in_even", [D, 1536]); pool_w = din("pool_w", [4, 128, 128]); pool_scale = din("pool_scale", [512])
    conv_b_w = din("conv_b_w", [31, 512]); conv_b_bias = din("conv_b_bias", [512])
    conv_ln_g = din("conv_ln_g", [512]); conv_ln_b = din("conv_ln_b", [512])
    w_out_even = din("w_out_even", [D, D]); ln_mix_even_g = din("ln_mix_even_g", [D]); ln_mix_even_b = din("ln_mix_even_b", [D])
    ffn_w1 = din("ffn_w1", [D, DFF]); ffn_w3 = din("ffn_w3", [D, DFF]); ffn_w2 = din("ffn_w2", [DFF, D])
    ln_ffn_even_g = din("ln_ffn_even_g", [D]); ln_ffn_even_b = din("ln_ffn_even_b", [D])
    w_in_odd = din("w_in_odd", [D, ODD_IN]); forget_bias = din("forget_bias", [8]); conv_d_w = din("conv_d_w", [3, 512])
    w_out_odd = din("w_out_odd", [D, D]); ln_mix_odd_g = din("ln_mix_odd_g", [D]); ln_mix_odd_b = din("ln_mix_odd_b", [D])
    router_w = din("router_w", [D, NE]); moe_w1 = din("moe_w1", [NE, D, DFE]); moe_w3 = din("moe_w3", [NE, D, DFE])
    moe_w2 = din("moe_w2", [NE, DFE, D]); ln_ffn_odd_g = din("ln_ffn_odd_g", [D]); ln_ffn_odd_b = din("ln_ffn_odd_b", [D])

    y_own = dout("y_own", [HALF, D]); y_s = dout("y_s", [NS, D])
    pool_tail = dout("pool_tail", [15, 512]); pool_s = dout("pool_s", [NS, 15, 512])
    convb_tail = dout("convb_tail", [30, 512]); convb_s = dout("convb_s", [NS, 30, 512])
    k_own = dout("k_own", [HALF, 512]); k_s = dout("k_s", [NS, 512])
    v_own = dout("v_own", [HALF, 512]); v_s = dout("v_s", [NS, 512])
    lf_own = dout("lf_own", [HALF, 8]); lf_s = dout("lf_s", [NS, 8])
    convd_tail = dout("convd_tail", [2, 512]); convd_s = dout("convd_s", [NS, 2, 512])

    xm_scr = dscr("xm_scr", [SEQ + NS, D]); x1_scr = dscr("x1_scr", [SEQ + NS, D]); x2_scr = dscr("x2_scr", [HALF + NS, D])

    G = es
    ident_f = sb(G, "ident_f", [128, 128]); ident_b = sb(G, "ident_b", [128, 128], BF16)
    ones_f = sb(G, "ones_f", [128, 128]); epsb = sb(G, "epsb", [128, 1])
    S.op("pool", lambda: POOL.memset(ones_f[:], 1.0), writes=[ones_f])
    S.op("pool", lambda: POOL.memset(epsb[:], EPS), writes=[epsb])
    S.op("pool", lambda: POOL.affine_select(out=ident_f[:], in_=ones_f[:], pattern=[[1, 128]], compare_op=ALU.is_equal,
                                             fill=0.0, base=0, channel_multiplier=-1), reads=[ones_f], writes=[ident_f])
    S.op("pool", lambda: POOL.tensor_copy(out=ident_b[:], in_=ident_f[:]), reads=[ident_f], writes=[ident_b])

    pbank = [ps(G, "pb%d" % i, [128, 512]) for i in range(7)]
    ptr = ps(G, "ptr", [128, 1024], BF16)
    pb_i = [0]

    def bank():
        b = pbank[pb_i[0] % 7]
        pb_i[0] += 1
        return b

    def load_w_bf(dst, dst_ap, src_ap):
        S.dma("pool", lambda: POOL.dma_start(out=dst_ap, in_=src_ap), writes=[dst])

    def load(eng, dst, dst_ap, src_ap):
        e = S.engs[eng]
        S.dma(eng, lambda: e.dma_start(out=dst_ap, in_=src_ap), writes=[dst])

    def store(eng, src, dst_ap, src_ap):
        return S.dma("pool", lambda: POOL.dma_start(out=dst_ap, in_=src_ap), reads=[src])

    def mm(pbuf, out_ap, pairs, reads):
        n = len(pairs)
        for i, (l, r) in enumerate(pairs):
            S.op("pe", lambda l=l, r=r, i=i: PE.matmul(out_ap, lhsT=l, rhs=r, start=(i == 0), stop=(i == n - 1)),
                 reads=reads, writes=[pbuf])

    def transpose(pbuf, out_ap, in_ap, idn, reads):
        S.op("pe", lambda: PE.transpose(out=out_ap, in_=in_ap, identity=idn), reads=reads, writes=[pbuf])

    def layernorm_rows(st_, r, nrows, gB, bB, out, tag):
        st_ = lnsets[lnrot[0] % 3]; lnrot[0] += 1
        stats = st_["stats"]; mv = st_["mv"]; rstd = st_["rstd"]
        for h in range(2):
            S.op("dve", lambda h=h: DVE.bn_stats(out=stats[:nrows, h, :], in_=r[:nrows, h * 512:(h + 1) * 512]),
                 reads=[r], writes=[stats])
        S.op("dve", lambda: DVE.bn_aggr(out=mv[:nrows, :], in_=stats[:nrows, :, :]), reads=[stats], writes=[mv])
        S.op("act", lambda: ACT.activation(out=rstd[:nrows, :], in_=mv[:nrows, 1:2], func=AF.Sqrt, bias=epsb[:nrows, :], scale=1.0),
             reads=[mv, epsb], writes=[rstd])
        S.op("dve", lambda: DVE.reciprocal(out=rstd[:nrows, :], in_=rstd[:nrows, :]), reads=[rstd], writes=[rstd])
        S.op("dve", lambda: DVE.tensor_scalar(out=out[:nrows, :], in0=r[:nrows, :], scalar1=mv[:nrows, 0:1], scalar2=rstd[:nrows, 0:1],
                                              op0=ALU.subtract, op1=ALU.mult), reads=[r, mv, rstd], writes=[out])
        S.op("pool", lambda: POOL.tensor_tensor(out=out[:nrows, :], in0=out[:nrows, :], in1=gB[:nrows, :], op=ALU.mult),
             reads=[out, gB], writes=[out])
        S.op("pool", lambda: POOL.tensor_tensor(out=out[:nrows, :], in0=out[:nrows, :], in1=bB[:nrows, :], op=ALU.add),
             reads=[out, bB], writes=[out])

    def to_fm(xtok, nrows, xbf, xT, col0, ncols_total_view=None):
        S.op("pool", lambda: POOL.tensor_copy(out=xbf[:nrows, :], in_=xtok[:nrows, :]), reads=[xtok], writes=[xbf])
        for k in range(8):
            transpose(ptr, ptr[:, k * 128:k * 128 + nrows], xbf[:nrows, k * 128:(k + 1) * 128], ident_b[:nrows, :nrows], [xbf, ident_b])
        S.op("act", lambda: ACT.copy(out=xT[:, :, col0:col0 + nrows],
                                     in_=ptr[:, :].rearrange("p (k t) -> p k t", k=8)[:, :, 0:nrows]), reads=[ptr], writes=[xT])

    lnsets = [{"stats": sb(G, "ln_stats%d" % q_, [128, 2, 6]), "mv": sb(G, "ln_mv%d" % q_, [128, 2]), "rstd": sb(G, "ln_rstd%d" % q_, [128, 1])}
              for q_ in range(3)]
    lnrot = [0]
    lnst = lnsets[0]

    with ExitStack() as P1:
        w_in = sb(P1, "w_in", [128, 8, 1536], BF16)
        w_out = sb(P1, "w_out", [128, 8, 1024], BF16)
        poolw = sb(P1, "poolw", [128, 4, 128], BF16)
        load_w_bf(w_in, w_in[:], w_in_even.rearrange("(c p) n -> p c n", p=128))
        load_w_bf(w_out, w_out[:], w_out_even.rearrange("(c p) n -> p c n", p=128))
        load_w_bf(poolw, poolw[:], pool_w.rearrange("g k m -> k g m"))
        vecs = sb(P1, "vecs", [128, 4, 4])
        for i, v in enumerate((pool_scale, conv_b_bias, conv_ln_g, conv_ln_b)):
            S.dma("sp", lambda i=i, v=v: SP.dma_start(out=vecs[:, i, :], in_=v.rearrange("(c p) -> p c", p=128),
                                                        allow_slow_non_contiguous=True), writes=[vecs])
        gB = sb(P1, "gB", [128, D]); bB = sb(P1, "bB", [128, D])
        load("sp", gB, gB[:], ln_mix_even_g.partition_broadcast(128))
        load("sp", bB, bB[:], ln_mix_even_b.partition_broadcast(128))
        cw_tok = sb(P1, "cw_tok", [31, 512]); convw = sb(P1, "convw", [128, 4, 31])
        load("sp", cw_tok, cw_tok[:], conv_b_w)
        pb = bank()
        for c in range(4):
            transpose(pb, pb[:, c * 31:(c + 1) * 31], cw_tok[:, c * 128:(c + 1) * 128], ident_f[:31, :31], [cw_tok, ident_f])
        S.op("dve", lambda: DVE.tensor_copy(out=convw[:], in_=pb[:, 0:124].rearrange("p (c j) -> p c j", c=4)),
             reads=[pb], writes=[convw])
        xbf = sb(P1, "xbf", [128, D], BF16); sg = sb(P1, "sg", [128, 512]); pooled = sb(P1, "pooled", [128, 512], BF16)
        cf = sb(P1, "cf", [128, 4, 512]); sq = sb(P1, "sq", [128, 4, 512])
        mean = sb(P1, "mean", [128, 512]); rs = sb(P1, "rs", [128, 512]); tmp = sb(P1, "tmp", [128, 512])
        r = sb(P1, "r", [128, D]); xos = [sb(P1, "xo%d" % q_, [128, D]) for q_ in range(2)]; xo = xos[0]
        P1p = ExitStack()
        diag = sb(P1p, "diag", [128, 4, 31, 128], BF16)
        for c in range(4):
            for j in range(31):
                S.op("dve", lambda c=c, j=j: DVE.tensor_scalar(out=diag[:, c, j, :], in0=ident_f[:], scalar1=convw[:, c, j:j + 1],
                                                               scalar2=None, op0=ALU.mult), reads=[ident_f, convw], writes=[diag])
        xtoks = [sb(P1p, "xtok%d" % q_, [128, 4, D]) for q_ in range(2)]
        xT = sb(P1p, "xT", [128, 8, 512], BF16)
        aT = sb(P1p, "aT", [128, 4, 528]); u32 = sb(P1p, "u32", [128, 4, 544]); ubf = sb(P1p, "ubf", [128, 4, 544], BF16)
        t1 = sb(P1p, "t1", [128, 528]); t2 = sb(P1p, "t2", [128, 528])
        invc = sb(P1p, "invc", [128, 4, 512])
        yT = sb(P1p, "yT", [128, 8, 512], BF16)
        for b_ in (aT, u32, ubf):
            S.op("pool", lambda b_=b_: POOL.memset(b_[:], 0.0), writes=[b_])

        def mixer_core(ncol, xTv, a_dst, sg_v, u_dst, ub_dst):
            for m in list(range(0, 4)) + [x for c in range(4) for x in (8 + c, 4 + c)]:
                pb = bank()
                mm(pb, pb[:, :ncol], [(w_in[:, k, m * 128:(m + 1) * 128], xTv(k)) for k in range(8)], [w_in, xT_cur[0]])
                if m < 4:
                    S.op("act", lambda m=m, pb=pb: ACT.copy(out=a_dst(m), in_=pb[:, :ncol]), reads=[pb], writes=[a_cur[0]])
                elif m >= 8:
                    S.op("act", lambda pb=pb: ACT.activation(out=sg_v, in_=pb[:, :ncol], func=AF.Sigmoid), reads=[pb], writes=[sg])
                else:
                    c = m - 4
                    S.op("dve", lambda c=c, pb=pb: DVE.tensor_tensor(out=u_dst(c), in0=pb[:, :ncol], in1=sg_v, op=ALU.mult),
                         reads=[pb, sg], writes=[u_cur[0]])
                    if ub_dst is not None:
                        S.op("pool", lambda c=c: POOL.tensor_copy(out=ub_dst(c), in_=u_dst(c)), reads=[u_cur[0]], writes=[ubf])

        def conv_ln_silu(ncol, cfv, sqv, y_dst, ybuf):
            for c in range(4):
                S.op("act", lambda c=c: ACT.activation(out=sqv(c), in_=cfv(c), func=AF.Square), reads=[cf], writes=[sq])
            p1 = bank(); p2 = bank()
            mm(p1, p1[:, :ncol], [(ones_f[:], cfv(c)) for c in range(4)], [ones_f, cf])
            mm(p2, p2[:, :ncol], [(ones_f[:], sqv(c)) for c in range(4)], [ones_f, sq])
            S.op("act", lambda: ACT.mul(out=mean[:, :ncol], in_=p1[:, :ncol], mul=1.0 / 512), reads=[p1], writes=[mean])
            S.op("dve", lambda: DVE.tensor_tensor(out=tmp[:, :ncol], in0=mean[:, :ncol], in1=mean[:, :ncol], op=ALU.mult),
                 reads=[mean], writes=[tmp])
            S.op("dve", lambda: DVE.scalar_tensor_tensor(out=rs[:, :ncol], in0=p2[:, :ncol], scalar=1.0 / 512, in1=tmp[:, :ncol],
                                                         op0=ALU.mult, op1=ALU.subtract), reads=[p2, tmp], writes=[rs])
            S.op("act", lambda: ACT.activation(out=rs[:, :ncol], in_=rs[:, :ncol], func=AF.Sqrt, bias=epsb[:, :], scale=1.0),
                 reads=[rs, epsb], writes=[rs])
            S.op("dve", lambda: DVE.reciprocal(out=rs[:, :ncol], in_=rs[:, :ncol]), reads=[rs], writes=[rs])
            for c in range(4):
                S.op("dve", lambda c=c: DVE.tensor_tensor(out=tmp[:, :ncol], in0=cfv(c), in1=mean[:, :ncol], op=ALU.subtract),
                     reads=[cf, mean], writes=[tmp])
                S.op("dve", lambda: DVE.tensor_tensor(out=tmp[:, :ncol], in0=tmp[:, :ncol], in1=rs[:, :ncol], op=ALU.mult),
                     reads=[tmp, rs], writes=[tmp])
                S.op("act", lambda c=c: ACT.activation(out=y_dst(c), in_=tmp[:, :ncol], func=AF.Silu,
                                                       bias=vecs[:, 3, c:c + 1], scale=vecs[:, 2, c:c + 1]),
                     reads=[tmp, vecs], writes=[ybuf])

        xT_cur = [xT]; a_cur = [aT]; u_cur = [u32]
        for it in range(SEQ // 512):
            t0 = it * 512
            xtok = xtoks[it % 2]
            load("sp", xtok, xtok[:, :, :], xloc[t0:t0 + 512, :].rearrange("(t p) d -> p t d", p=128))
            for g in range(4):
                load("sp", invc, invc[:, g, :], invcnt[g, t0:t0 + 512].partition_broadcast(128))
            for tt in range(4):
                S.op("pool", lambda tt=tt: POOL.tensor_copy(out=xbf[:, :], in_=xtok[:, tt, :]), reads=[xtok], writes=[xbf])
                for k in range(8):
                    transpose(ptr, ptr[:, k * 128:(k + 1) * 128], xbf[:, k * 128:(k + 1) * 128], ident_b[:], [xbf, ident_b])
                S.op("act", lambda tt=tt: ACT.copy(out=xT[:, :, tt * 128:(tt + 1) * 128],
                                                   in_=ptr[:, :].rearrange("p (k t) -> p k t", k=8)), reads=[ptr], writes=[xT])
            mixer_core(512, lambda k: xT[:, k, :], lambda m: aT[:, m, 16:528], sg[:, :], lambda c: u32[:, c, 32:544],
                       lambda c: ubf[:, c, 32:544])
            for g in range(4):
                A = aT
                S.op("dve", lambda g=g: DVE.tensor_tensor(out=t1[:, 1:528], in0=aT[:, g, 1:528], in1=aT[:, g, 0:527], op=ALU.add),
                     reads=[aT], writes=[t1])
                win = t1
                if g >= 1:
                    S.op("dve", lambda: DVE.tensor_tensor(out=t2[:, 3:528], in0=t1[:, 3:528], in1=t1[:, 1:526], op=ALU.add),
                         reads=[t1], writes=[t2])
                    win = t2
                if g >= 2:
                    S.op("dve", lambda: DVE.tensor_tensor(out=t1[:, 7:528], in0=t2[:, 7:528], in1=t2[:, 3:524], op=ALU.add),
                         reads=[t2], writes=[t1])
                    win = t1
                if g >= 3:
                    S.op("dve", lambda: DVE.tensor_tensor(out=t2[:, 15:528], in0=t1[:, 15:528], in1=t1[:, 7:520], op=ALU.add),
                         reads=[t1], writes=[t2])
                    win = t2
                S.op("dve", lambda g=g, win=win: DVE.tensor_tensor(out=tmp[:, :], in0=win[:, 16:528], in1=invc[:, g, :], op=ALU.mult),
                     reads=[win, invc], writes=[tmp])
                S.op("dve", lambda g=g: DVE.tensor_tensor(out=pooled[:, :], in0=tmp[:, :], in1=aT[:, g, 16:528], op=ALU.subtract),
                     reads=[tmp, aT], writes=[pooled])
                pb = bank()
                mm(pb, pb[:, :], [(poolw[:, g, :], pooled[:, :])], [poolw, pooled])
                S.op("act", lambda g=g, pb=pb: ACT.activation(out=yT[:, g, :], in_=pb[:, :], func=AF.Identity, scale=vecs[:, 0, g:g + 1]),
                     reads=[pb, vecs], writes=[yT])
            for c in range(4):
                pb = bank()
                mm(pb, pb[:, :], [(diag[:, c, j, :], ubf[:, c, 2 + j:2 + j + 512]) for j in range(31)], [diag, ubf])
                S.op("act", lambda c=c, pb=pb: ACT.activation(out=cf[:, c, :], in_=pb[:, :], func=AF.Identity,
                                                              bias=vecs[:, 1, c:c + 1], scale=1.0), reads=[pb, vecs], writes=[cf])
            conv_ln_silu(512, lambda c: cf[:, c, :], lambda c: sq[:, c, :], lambda c: yT[:, 4 + c, :], yT)
            for tt in range(4):
                p1 = bank(); p2 = bank()
                for hf, pb in ((0, p1), (1, p2)):
                    mm(pb, pb[:, :], [(yT[:, k, tt * 128:(tt + 1) * 128], w_out[:, k, hf * 512:(hf + 1) * 512]) for k in range(8)],
                       [yT, w_out])
                    S.op("dve", lambda tt=tt, hf=hf, pb=pb: DVE.scalar_tensor_tensor(
                        out=r[:, hf * 512:(hf + 1) * 512], in0=xtok[:, tt, hf * 512:(hf + 1) * 512], scalar=ALPHA, in1=pb[:, :],
                        op0=ALU.mult, op1=ALU.add), reads=[xtok, pb], writes=[r])
                xo = xos[tt % 2]
                layernorm_rows(lnst, r, 128, gB, bB, xo, "p1")
                store("sp", xo, xm_scr[t0 + tt * 128:t0 + (tt + 1) * 128, :], xo[:, :])
            S.op("pool", lambda: POOL.tensor_copy(out=aT[:, :, 1:16], in_=aT[:, :, 513:528]), reads=[aT], writes=[aT])
            S.op("pool", lambda: POOL.tensor_copy(out=u32[:, :, 2:32], in_=u32[:, :, 514:544]), reads=[u32], writes=[u32])
            S.op("pool", lambda: POOL.tensor_copy(out=ubf[:, :, 2:32], in_=ubf[:, :, 514:544]), reads=[ubf], writes=[ubf])
        tail = sb(P1p, "tail", [32, 512])
        pb = bank()
        for c in range(4):
            transpose(pb, pb[:15, c * 128:(c + 1) * 128], aT[:, c, 1:16], ident_f[:], [aT, ident_f])
        S.op("dve", lambda: DVE.tensor_copy(out=tail[:15, :], in_=pb[:15, :]), reads=[pb], writes=[tail])
        store("sp", tail, pool_tail, tail[:15, :])
        pb = bank()
        for c in range(4):
            transpose(pb, pb[:30, c * 128:(c + 1) * 128], u32[:, c, 2:32], ident_f[:], [u32, ident_f])
        S.op("dve", lambda: DVE.tensor_copy(out=tail[:30, :], in_=pb[:30, :]), reads=[pb], writes=[tail])
        store("sp", tail, convb_tail, tail[:30, :])

        S.barrier()
        P1p.close()
        xs_tok = sb(P1, "xs_tok", [NS, D]); xsT = sb(P1, "xsT", [128, 8, NS], BF16)
        aTs = sb(P1, "aTs", [128, 4, NS, 16]); uTs = sb(P1, "uTs", [128, 4, NS, 31])
        st_tok = sb(P1, "st_tok", [120, 512])
        load("sp", xs_tok, xs_tok[:], xs)
        to_fm(xs_tok, NS, xbf, xsT, 0)
        load("sp", st_tok, st_tok[:60, :], spool)
        pb = bank()
        for c in range(4):
            transpose(pb, pb[:, c * 60:(c + 1) * 60], st_tok[:60, c * 128:(c + 1) * 128], ident_f[:60, :60], [st_tok, ident_f])
        S.op("dve", lambda: DVE.tensor_copy(out=aTs[:, :, :, 0:15], in_=pb[:, 0:240].rearrange("p (c b j) -> p c b j", c=4, b=NS)),
             reads=[pb], writes=[aTs])
        load("sp", st_tok, st_tok[:120, :], sconvb)
        pb = bank()
        for c in range(4):
            transpose(pb, pb[:, c * 120:(c + 1) * 120], st_tok[:120, c * 128:(c + 1) * 128], ident_f[:120, :120], [st_tok, ident_f])
        S.op("dve", lambda: DVE.tensor_copy(out=uTs[:, :, :, 0:30], in_=pb[:, 0:480].rearrange("p (c b j) -> p c b j", c=4, b=NS)),
             reads=[pb], writes=[uTs])
        xT_cur[0] = xsT; a_cur[0] = aTs; u_cur[0] = uTs
        mixer_core(NS, lambda k: xsT[:, k, :], lambda m: aTs[:, m, :, 15], sg[:, :NS], lambda c: uTs[:, c, :, 30], None)
        yTs = sb(P1, "yTs", [128, 8, NS], BF16)
        wsum = sb(P1, "wsum", [128, NS])
        for g, w in enumerate((2, 4, 8, 16)):
            S.op("dve", lambda g=g, w=w: DVE.tensor_reduce(out=wsum[:, :], in_=aTs[:, g, :, 16 - w:16], axis=AX.X, op=ALU.add),
                 reads=[aTs], writes=[wsum])
            S.op("dve", lambda g=g, w=w: DVE.scalar_tensor_tensor(out=pooled[:, :NS], in0=wsum[:, :], scalar=1.0 / w, in1=aTs[:, g, :, 15],
                                                                   op0=ALU.mult, op1=ALU.subtract), reads=[wsum, aTs], writes=[pooled])
            pb = bank()
            mm(pb, pb[:, :NS], [(poolw[:, g, :], pooled[:, :NS])], [poolw, pooled])
            S.op("act", lambda g=g, pb=pb: ACT.activation(out=yTs[:, g, :], in_=pb[:, :NS], func=AF.Identity, scale=vecs[:, 0, g:g + 1]),
                 reads=[pb, vecs], writes=[yTs])
        prod = sb(P1, "prod", [128, NS, 31])
        for c in range(4):
            S.op("dve", lambda c=c: DVE.tensor_tensor(out=prod[:, :, :], in0=uTs[:, c, :, :],
                                                      in1=convw[:, c:c + 1, :].to_broadcast([128, NS, 31]), op=ALU.mult),
                 reads=[uTs, convw], writes=[prod])
            S.op("dve", lambda: DVE.tensor_reduce(out=wsum[:, :], in_=prod[:, :, :], axis=AX.X, op=ALU.add), reads=[prod], writes=[wsum])
            S.op("act", lambda c=c: ACT.activation(out=cf[:, c, :NS], in_=wsum[:, :], func=AF.Identity, bias=vecs[:, 1, c:c + 1], scale=1.0),
                 reads=[wsum, vecs], writes=[cf])
        conv_ln_silu(NS, lambda c: cf[:, c, :NS], lambda c: sq[:, c, :NS], lambda c: yTs[:, 4 + c, :], yTs)
        p1 = bank(); p2 = bank()
        for hf, pb in ((0, p1), (1, p2)):
            mm(pb, pb[:NS, :], [(yTs[:, k, :], w_out[:, k, hf * 512:(hf + 1) * 512]) for k in range(8)], [yTs, w_out])
            S.op("dve", lambda hf=hf, pb=pb: DVE.scalar_tensor_tensor(
                out=r[:NS, hf * 512:(hf + 1) * 512], in0=xs_tok[:, hf * 512:(hf + 1) * 512], scalar=ALPHA, in1=pb[:NS, :],
                op0=ALU.mult, op1=ALU.add), reads=[xs_tok, pb], writes=[r])
        layernorm_rows(lnst, r, NS, gB, bB, xo, "p1s")
        store("sp", xo, xm_scr[SEQ:SEQ + NS, :], xo[:NS, :])
        new_tok = sb(P1, "new_tok", [NS, 1536])
        for n3 in range(3):
            pb = bank()
            mm(pb, pb[:NS, :], [(xsT[:, k, :], w_in[:, k, n3 * 512:(n3 + 1) * 512]) for k in range(8)], [xsT, w_in])
            if n3 < 2:
                S.op("act", lambda n3=n3, pb=pb: ACT.copy(out=new_tok[:, n3 * 512:(n3 + 1) * 512], in_=pb[:NS, :]), reads=[pb], writes=[new_tok])
            else:
                S.op("act", lambda pb=pb: ACT.activation(out=new_tok[:, 1024:1536], in_=pb[:NS, :], func=AF.Sigmoid), reads=[pb], writes=[new_tok])
        S.op("dve", lambda: DVE.tensor_tensor(out=new_tok[:, 512:1024], in0=new_tok[:, 512:1024], in1=new_tok[:, 1024:1536], op=ALU.mult),
             reads=[new_tok], writes=[new_tok])
        store("sp", new_tok, pool_s[:, 14, :], new_tok[:, 0:512])
        store("sp", new_tok, convb_s[:, 29, :], new_tok[:, 512:1024])
        dummy = Buf(None)
        S.dma("sp", lambda: SP.dma_start(out=pool_s[:, 0:14, :], in_=spool.rearrange("(b j) c -> b j c", b=NS)[:, 1:15, :]))
        S.dma("sp", lambda: SP.dma_start(out=convb_s[:, 0:29, :], in_=sconvb.rearrange("(b j) c -> b j c", b=NS)[:, 1:30, :]))
        S.barrier()

    S.barrier()

    def gated_pass(name, src_rows, experts, lng, lnb, dst_rows, router=None):
        NTL = len(src_rows)
        cols = []
        c0 = 0
        for (_, n) in src_rows:
            cols.append((c0, n)); c0 += n
        NT = c0
        ntiles = [(c, min(512, NT - c)) for c in range(0, NT, 512)]
        with ExitStack() as P:
            xT2 = sb(P, name + "xT", [128, 8, NT], BF16)
            acc = sb(P, name + "acc", [128, NTL, D])
            hT = sb(P, name + "hT", [128, 4, NT], BF16)
            xts = [sb(P, name + "xt%d" % q_, [128, D]) for q_ in range(3)]; xbs = [sb(P, name + "xb%d" % q_, [128, D], BF16) for q_ in range(2)]
            sil = sb(P, name + "sil", [128, 512])
            gB2 = sb(P, name + "gB", [128, D]); bB2 = sb(P, name + "bB", [128, D])
            load("sp", gB2, gB2[:], lng.partition_broadcast(128))
            load("sp", bB2, bB2[:], lnb.partition_broadcast(128))
            wset = [(sb(P, name + "w1_%d" % i, [128, 8, 512], BF16), sb(P, name + "w3_%d" % i, [128, 8, 512], BF16),
                     sb(P, name + "w2_%d" % i, [128, 4, D], BF16)) for i in range(2)]
            gate = None
            if router is not None:
                gate = sb(P, name + "gate", [128, NTL, NE])
                rw = sb(P, name + "rw", [128, 8, NE]); xTf = sb(P, name + "xTf", [128, 8, 128])
                lg = sb(P, name + "lg", [128, NE]); m8 = sb(P, name + "m8", [128, 8]); g12 = sb(P, name + "g12", [128, 4])
                ga = sb(P, name + "ga", [128, NE])
                S.dma("sp", lambda: SP.dma_start(out=rw[:], in_=router.rearrange("(c p) e -> p c e", p=128)), writes=[rw])
            groups = []
            for (w1a, w3a, w2a, F, ge) in experts:
                for f0 in range(0, F, 512):
                    groups.append((w1a, w3a, w2a, f0, min(512, F - f0), ge))

            def issue_w(g):
                if g >= len(groups):
                    return
                (w1a, w3a, w2a, f0, fw, ge) = groups[g]
                w1g, w3g, w2g = wset[g % 2]
                load_w_bf(w1g, w1g[:, :, :fw], w1a[:, f0:f0 + fw].rearrange("(c p) n -> p c n", p=128))
                load_w_bf(w3g, w3g[:, :, :fw], w3a[:, f0:f0 + fw].rearrange("(c p) n -> p c n", p=128))
                load_w_bf(w2g, w2g[:, :fw // 128, :], w2a[f0:f0 + fw, :].rearrange("(c p) n -> p c n", p=128))
            issue_w(0); issue_w(1)
            for j, (src, n) in enumerate(src_rows):
                xt = xts[j % 3]; xb2 = xbs[j % 2]
                load("sp", xt, xt[:n, :], src)
                S.op("act", lambda j=j, n=n: ACT.mul(out=acc[:n, j, :], in_=xt[:n, :], mul=ALPHA), reads=[xt], writes=[acc])
                to_fm(xt, n, xb2, xT2, cols[j][0])
                if router is not None:
                    p1 = bank(); p2 = bank()
                    for k in range(8):
                        pb = p1 if k < 4 else p2
                        transpose(pb, pb[:, (k % 4) * 128:(k % 4) * 128 + n], xt[:n, k * 128:(k + 1) * 128], ident_f[:n, :n], [xt, ident_f])
                    S.op("dve", lambda n=n: DVE.tensor_copy(out=xTf[:, 0:4, :n], in_=p1[:, :].rearrange("p (k t) -> p k t", k=4)[:, :, :n]),
                         reads=[p1], writes=[xTf])
                    S.op("dve", lambda n=n: DVE.tensor_copy(out=xTf[:, 4:8, :n], in_=p2[:, :].rearrange("p (k t) -> p k t", k=4)[:, :, :n]),
                         reads=[p2], writes=[xTf])
                    pb = bank()
                    mm(pb, pb[:n, :NE], [(xTf[:, k, :n], rw[:, k, :]) for k in range(8)], [xTf, rw])
                    S.op("dve", lambda n=n, pb=pb: DVE.tensor_copy(out=lg[:n, :], in_=pb[:n, :NE]), reads=[pb], writes=[lg])
                    S.op("dve", lambda n=n: DVE.max(out=m8[:n, :], in_=lg[:n, :]), reads=[lg], writes=[m8])
                    S.op("dve", lambda n=n: DVE.tensor_tensor(out=g12[:n, 0:1], in0=m8[:n, 1:2], in1=m8[:n, 0:1], op=ALU.subtract),
                         reads=[m8], writes=[g12])
                    S.op("act", lambda n=n: ACT.activation(out=g12[:n, 1:2], in_=g12[:n, 0:1], func=AF.Exp), reads=[g12], writes=[g12])
                    S.op("dve", lambda n=n: DVE.tensor_scalar(out=g12[:n, 1:2], in0=g12[:n, 1:2], scalar1=1.0, scalar2=None, op0=ALU.add),
                         reads=[g12], writes=[g12])
                    S.op("dve", lambda n=n: DVE.reciprocal(out=g12[:n, 2:3], in_=g12[:n, 1:2]), reads=[g12], writes=[g12])
                    S.op("dve", lambda n=n: DVE.tensor_scalar(out=g12[:n, 3:4], in0=g12[:n, 2:3], scalar1=-1.0, scalar2=1.0,
                                                              op0=ALU.mult, op1=ALU.add), reads=[g12], writes=[g12])
                    S.op("dve", lambda n=n: DVE.tensor_scalar(out=ga[:n, :], in0=lg[:n, :], scalar1=m8[:n, 0:1], scalar2=g12[:n, 2:3],
                                                              op0=ALU.is_equal, op1=ALU.mult), reads=[lg, m8, g12], writes=[ga])
                    S.op("dve", lambda n=n, j=j: DVE.tensor_scalar(out=gate[:n, j, :], in0=lg[:n, :], scalar1=m8[:n, 1:2], scalar2=g12[:n, 3:4],
                                                                   op0=ALU.is_equal, op1=ALU.mult), reads=[lg, m8, g12], writes=[gate])
                    S.op("dve", lambda n=n, j=j: DVE.tensor_tensor(out=gate[:n, j, :], in0=gate[:n, j, :], in1=ga[:n, :], op=ALU.add),
                         reads=[gate, ga], writes=[gate])
            for gi, (w1a, w3a, w2a, f0, fw, ge) in enumerate(groups):
                if True:
                    nfc = fw // 128
                    w1g, w3g, w2g = wset[gi % 2]
                    for fc in range(nfc):
                        for (cc, n) in ntiles:
                            p1 = bank(); p3 = bank()
                            mm(p1, p1[:, :n], [(w1g[:, k, fc * 128:(fc + 1) * 128], xT2[:, k, cc:cc + n]) for k in range(8)], [w1g, xT2])
                            mm(p3, p3[:, :n], [(w3g[:, k, fc * 128:(fc + 1) * 128], xT2[:, k, cc:cc + n]) for k in range(8)], [w3g, xT2])
                            S.op("act", lambda p1=p1, n=n: ACT.activation(out=sil[:, :n], in_=p1[:, :n], func=AF.Silu), reads=[p1], writes=[sil])
                            S.op("dve", lambda p3=p3, n=n, fc=fc, cc=cc: DVE.tensor_tensor(out=hT[:, fc, cc:cc + n], in0=p3[:, :n], in1=sil[:, :n],
                                                                                        op=ALU.mult), reads=[p3, sil], writes=[hT])
                    for j, (cc, n) in enumerate(cols):
                        for hf in range(2):
                            pb = bank()
                            mm(pb, pb[:n, :], [(hT[:, fc, cc:cc + n], w2g[:, fc, hf * 512:(hf + 1) * 512]) for fc in range(nfc)], [hT, w2g])
                            if ge is None:
                                S.op("dve", lambda pb=pb, n=n, j=j, hf=hf: DVE.tensor_tensor(
                                    out=acc[:n, j, hf * 512:(hf + 1) * 512], in0=pb[:n, :], in1=acc[:n, j, hf * 512:(hf + 1) * 512], op=ALU.add),
                                    reads=[pb, acc], writes=[acc])
                            else:
                                S.op("dve", lambda pb=pb, n=n, j=j, hf=hf, ge=ge: DVE.scalar_tensor_tensor(
                                    out=acc[:n, j, hf * 512:(hf + 1) * 512], in0=pb[:n, :], scalar=gate[:n, j, ge:ge + 1],
                                    in1=acc[:n, j, hf * 512:(hf + 1) * 512], op0=ALU.mult, op1=ALU.add), reads=[pb, acc, gate], writes=[acc])
                    issue_w(gi + 2)
            for j, (dst, n) in enumerate(dst_rows):
                xt = xts[j % 3]
                S.op("pool", lambda j=j, n=n: POOL.tensor_copy(out=xt[:n, :], in_=acc[:n, j, :]), reads=[acc], writes=[xt])
                layernorm_rows(lnst, xt, n, gB2, bB2, xt, name)
                store("sp", xt, dst, xt[:n, :])
        S.barrier()

    def rows(t, a, b):
        return [(t[r:r + 128, :], 128) for r in range(a, b, 128)]

    ffn = [(ffn_w1, ffn_w3, ffn_w2, DFF, None)]
    gated_pass("f0", rows(xm_scr, 0, HALF), ffn, ln_ffn_even_g, ln_ffn_even_b, rows(x1_scr, 0, HALF))
    gated_pass("f1", rows(xm_scr, HALF, SEQ) + [(xm_scr[SEQ:SEQ + NS, :], NS)], ffn, ln_ffn_even_g, ln_ffn_even_b,
               rows(x1_scr, HALF, SEQ) + [(x1_scr[SEQ:SEQ + NS, :], NS)])

    with ExitStack() as P3:
        KT = sb(P3, "KT", [128, 4, SEQ], BF16); QT = sb(P3, "QT", [128, 4, HALF], BF16)
        Vx = sb(P3, "Vx", [128, 32, 8, 65], BF16)
        yTd = sb(P3, "yTd", [128, 4, HALF], BF16)
        negFm = sb(P3, "negFm", [128, 32, 8]); Fend = sb(P3, "Fend", [128, 16, 8])
        pmk = sb(P3, "pmk", [128, 32]); hfl = sb(P3, "hfl", [128, 1]); one1 = sb(P3, "one1", [128, 1])
        fbB = sb(P3, "fbB", [128, 8]); tri = sb(P3, "tri", [128, 128]); sel127 = sb(P3, "sel127", [128, 128])
        maskT = sb(P3, "maskT", [128, 128], BF16); maskf = sb(P3, "maskf", [128, 128])
        carry = sb(P3, "carry", [128, 8]); cdw = sb(P3, "cdw", [128, 4, 3]); cdw_t = sb(P3, "cdw_t", [3, 512])
        load("sp", pmk, pmk[:], pmask); load("sp", hfl, hfl[:], hflag)
        load("sp", fbB, fbB[:], forget_bias.partition_broadcast(128))
        load("sp", cdw_t, cdw_t[:], conv_d_w)
        pb = bank()
        for c in range(4):
            transpose(pb, pb[:, c * 3:(c + 1) * 3], cdw_t[:, c * 128:(c + 1) * 128], ident_f[:3, :3], [cdw_t, ident_f])
        S.op("dve", lambda: DVE.tensor_copy(out=cdw[:], in_=pb[:, 0:12].rearrange("p (c j) -> p c j", c=4)), reads=[pb], writes=[cdw])
        S.op("pool", lambda: POOL.memset(one1[:], 1.0), writes=[one1])
        S.op("pool", lambda: POOL.memset(carry[:], 0.0), writes=[carry])
        S.op("pool", lambda: POOL.memset(Vx[:], 1.0), writes=[Vx])
        S.op("pool", lambda: POOL.affine_select(out=tri[:], in_=ones_f[:], pattern=[[1, 128]], compare_op=ALU.is_ge, fill=0.0,
                                                 base=0, channel_multiplier=-1), reads=[ones_f], writes=[tri])
        S.op("pool", lambda: POOL.affine_select(out=sel127[:], in_=ones_f[:], pattern=[[0, 128]], compare_op=ALU.is_equal, fill=0.0,
                                                 base=-127, channel_multiplier=1), reads=[ones_f], writes=[sel127])
        S.op("pool", lambda: POOL.memset(maskf[:], 0.0), writes=[maskf])
        S.op("pool", lambda: POOL.affine_select(out=maskf[:], in_=maskf[:], pattern=[[1, 128]], compare_op=ALU.is_ge, fill=NEG,
                                                 base=0, channel_multiplier=-1), reads=[maskf], writes=[maskf])
        S.op("pool", lambda: POOL.tensor_copy(out=maskT[:], in_=maskf[:]), reads=[maskf], writes=[maskT])

        qs_tok = sb(P3, "qs_tok", [NS, 512]); knew = sb(P3, "knew", [NS, 512]); vnew = sb(P3, "vnew", [NS, 512]); lfnew = sb(P3, "lfnew", [NS, 8])
        yTds = sb(P3, "yTds", [128, 4, NS], BF16); xs1 = sb(P3, "xs1", [NS, D])
        with ExitStack() as P3a:
            w_odd = sb(P3a, "w_odd", [128, 8, ODD_IN], BF16)
            load_w_bf(w_odd, w_odd[:], w_in_odd.rearrange("(c p) n -> p c n", p=128))
            x1ts = [sb(P3a, "x1t%d" % q_, [128, D]) for q_ in range(2)]; x1b = sb(P3a, "x1b", [128, D], BF16); x1T = sb(P3a, "x1T", [128, 8, 512], BF16)
            ktoks = [sb(P3a, "ktok%d" % q_, [128, 512]) for q_ in range(2)]; vtoks = [sb(P3a, "vtok%d" % q_, [128, 512]) for q_ in range(2)]; lft = sb(P3a, "lft", [128, 8]); ft = sb(P3a, "ft", [128, 8])
            hf32 = sb(P3a, "hf32", [128, 512]); ud = sb(P3a, "ud", [128, 4, 514]); yd = sb(P3a, "yd", [128, 512]); bg = sb(P3a, "bg", [128, 512])
            S.op("pool", lambda: POOL.memset(ud[:], 0.0), writes=[ud])

            def logf_from(psb, n, dst):
                S.op("dve", lambda: DVE.tensor_tensor(out=dst[:n, :], in0=psb[:n, 0:8], in1=fbB[:n, :], op=ALU.add), reads=[psb, fbB], writes=[dst])
                S.op("act", lambda: ACT.activation(out=dst[:n, :], in_=dst[:n, :], func=AF.Exp, scale=-1.0), reads=[dst], writes=[dst])
                S.op("act", lambda: ACT.activation(out=dst[:n, :], in_=dst[:n, :], func=AF.Ln, bias=one1[:n, :], scale=1.0), reads=[dst, one1], writes=[dst])
                S.op("dve", lambda: DVE.tensor_scalar(out=dst[:n, :], in0=dst[:n, :], scalar1=-1.0, scalar2=None, op0=ALU.mult), reads=[dst], writes=[dst])

            for it in range(SEQ // 512):
                t0 = it * 512
                own = it >= 4
                for tt in range(4):
                    x1t = x1ts[tt % 2]
                    load("sp", x1t, x1t[:, :], x1_scr[t0 + tt * 128:t0 + (tt + 1) * 128, :])
                    to_fm(x1t, 128, x1b, x1T, tt * 128)
                for c in range(4):
                    pb = bank()
                    mm(pb, pb[:, :], [(w_odd[:, k, 512 + c * 128:512 + (c + 1) * 128], x1T[:, k, :]) for k in range(8)], [w_odd, x1T])
                    S.op("act", lambda c=c, pb=pb, t0=t0: ACT.copy(out=KT[:, c, t0:t0 + 512], in_=pb[:, :]), reads=[pb], writes=[KT])
                    if own:
                        pb = bank()
                        mm(pb, pb[:, :], [(w_odd[:, k, c * 128:(c + 1) * 128], x1T[:, k, :]) for k in range(8)], [w_odd, x1T])
                        S.op("act", lambda c=c, pb=pb, t0=t0: ACT.mul(out=QT[:, c, t0 - HALF:t0 - HALF + 512], in_=pb[:, :], mul=0.125),
                             reads=[pb], writes=[QT])
                for tt in range(4):
                    tl = it * 4 + tt
                    r0 = t0 + tt * 128
                    pk = bank(); pv = bank(); pf = bank()
                    ktok = ktoks[tt % 2]; vtok = vtoks[tt % 2]
                    xs_ = [x1T[:, k, tt * 128:(tt + 1) * 128] for k in range(8)]
                    mm(pv, pv[:, :], [(xs_[k], w_odd[:, k, 1024:1536]) for k in range(8)], [x1T, w_odd])
                    mm(pf, pf[:, 0:8], [(xs_[k], w_odd[:, k, 1536:1544]) for k in range(8)], [x1T, w_odd])
                    S.op("act", lambda pv=pv, tl=tl: ACT.copy(out=Vx[:, tl, :, 0:64], in_=pv[:, :].rearrange("p (h d) -> p h d", h=8)),
                         reads=[pv], writes=[Vx])
                    logf_from(pf, 128, lft)
                    if own:
                        mm(pk, pk[:, :], [(xs_[k], w_odd[:, k, 512:1024]) for k in range(8)], [x1T, w_odd])
                        S.op("dve", lambda pk=pk: DVE.tensor_copy(out=ktok[:, :], in_=pk[:, :]), reads=[pk], writes=[ktok])
                        S.op("dve", lambda pv=pv: DVE.tensor_copy(out=vtok[:, :], in_=pv[:, :]), reads=[pv], writes=[vtok])
                        store("sp", ktok, k_own[r0 - HALF:r0 - HALF + 128, :], ktok[:, :])
                        store("sp", vtok, v_own[r0 - HALF:r0 - HALF + 128, :], vtok[:, :])
                        store("sp", lft, lf_own[r0 - HALF:r0 - HALF + 128, :], lft[:, :])
                    p1 = bank(); p2 = bank()
                    mm(p1, p1[:, 0:8], [(tri[:], lft[:, :])], [tri, lft])
                    mm(p2, p2[:, 0:8], [(ones_f[:], lft[:, :])], [ones_f, lft])
                    S.op("dve", lambda p1=p1: DVE.tensor_tensor(out=ft[:, :], in0=p1[:, 0:8], in1=carry[:, :], op=ALU.add), reads=[p1, carry], writes=[ft])
                    S.op("dve", lambda p2=p2: DVE.tensor_tensor(out=carry[:, :], in0=p2[:, 0:8], in1=carry[:, :], op=ALU.add), reads=[p2, carry], writes=[carry])
                    S.op("dve", lambda tl=tl: DVE.tensor_scalar(out=negFm[:, tl, :], in0=ft[:, :], scalar1=-1.0, scalar2=pmk[:, tl:tl + 1],
                                                                 op0=ALU.mult, op1=ALU.add), reads=[ft, pmk], writes=[negFm])
                    if own:
                        p3 = bank()
                        mm(p3, p3[:, 0:8], [(sel127[:], ft[:, :])], [sel127, ft])
                        S.op("dve", lambda p3=p3, tl=tl: DVE.tensor_copy(out=Fend[:, tl - 16, :], in_=p3[:, 0:8]), reads=[p3], writes=[Fend])
                if it >= 3:
                    for c in range(4):
                        ph = bank(); pc = bank()
                        mm(ph, ph[:, :], [(w_odd[:, k, 1544 + c * 128:1544 + (c + 1) * 128], x1T[:, k, :]) for k in range(8)], [w_odd, x1T])
                        mm(pc, pc[:, :], [(w_odd[:, k, 2568 + c * 128:2568 + (c + 1) * 128], x1T[:, k, :]) for k in range(8)], [w_odd, x1T])
                        S.op("act", lambda ph=ph: ACT.copy(out=hf32[:, :], in_=ph[:, :]), reads=[ph], writes=[hf32])
                        S.op("dve", lambda pc=pc, c=c: DVE.tensor_tensor(out=ud[:, c, 2:514], in0=pc[:, :], in1=hf32[:, :], op=ALU.mult),
                             reads=[pc, hf32], writes=[ud])
                        if own:
                            pg = bank()
                            mm(pg, pg[:, :], [(w_odd[:, k, 2056 + c * 128:2056 + (c + 1) * 128], x1T[:, k, :]) for k in range(8)], [w_odd, x1T])
                            S.op("act", lambda pg=pg: ACT.copy(out=bg[:, :], in_=pg[:, :]), reads=[pg], writes=[bg])
                            S.op("dve", lambda c=c: DVE.tensor_scalar(out=yd[:, :], in0=ud[:, c, 0:512], scalar1=cdw[:, c, 0:1], scalar2=None, op0=ALU.mult),
                                 reads=[ud, cdw], writes=[yd])
                            for j in (1, 2):
                                S.op("dve", lambda c=c, j=j: DVE.scalar_tensor_tensor(out=yd[:, :], in0=ud[:, c, j:j + 512], scalar=cdw[:, c, j:j + 1],
                                                                                     in1=yd[:, :], op0=ALU.mult, op1=ALU.add), reads=[ud, cdw, yd], writes=[yd])
                            S.op("dve", lambda c=c, t0=t0: DVE.tensor_tensor(out=yTd[:, c, t0 - HALF:t0 - HALF + 512], in0=yd[:, :], in1=bg[:, :], op=ALU.mult),
                                 reads=[yd, bg], writes=[yTd])
                    S.op("pool", lambda: POOL.tensor_copy(out=ud[:, :, 0:2], in_=ud[:, :, 512:514]), reads=[ud], writes=[ud])
                    if it == 3:
                        S.op("dve", lambda: DVE.tensor_scalar(out=ud[:, :, 0:2], in0=ud[:, :, 0:2], scalar1=hfl[:, 0:1], scalar2=None, op0=ALU.mult),
                             reads=[ud, hfl], writes=[ud])
            tl2 = sb(P3a, "tl2", [2, 512])
            pb = bank()
            for c in range(4):
                transpose(pb, pb[:2, c * 128:(c + 1) * 128], ud[:, c, 0:2], ident_f[:], [ud, ident_f])
            S.op("dve", lambda: DVE.tensor_copy(out=tl2[:, :], in_=pb[:2, :]), reads=[pb], writes=[tl2])
            store("sp", tl2, convd_tail, tl2[:, :])

            xs1T = sb(P3a, "xs1T", [128, 8, NS], BF16)
            load("sp", xs1, xs1[:, :], x1_scr[SEQ:SEQ + NS, :])
            to_fm(xs1, NS, x1b, xs1T, 0)
            for (off, dst, sc) in ((0, qs_tok, 0.125), (512, knew, 1.0), (1024, vnew, 1.0)):
                pb = bank()
                mm(pb, pb[:NS, :], [(xs1T[:, k, :], w_odd[:, k, off:off + 512]) for k in range(8)], [xs1T, w_odd])
                S.op("act", lambda pb=pb, dst=dst, sc=sc: ACT.mul(out=dst[:, :], in_=pb[:NS, :], mul=sc), reads=[pb], writes=[dst])
            pb = bank()
            mm(pb, pb[:NS, 0:8], [(xs1T[:, k, :], w_odd[:, k, 1536:1544]) for k in range(8)], [xs1T, w_odd])
            logf_from(pb, NS, lfnew)
            store("sp", knew, k_s, knew[:, :]); store("sp", vnew, v_s, vnew[:, :]); store("sp", lfnew, lf_s, lfnew[:, :])
            sd_tok = sb(P3a, "sd_tok", [NS * 2, 512]); uds = sb(P3a, "uds", [128, 4, NS, 3]); hs = sb(P3a, "hs", [128, NS]); bgs = sb(P3a, "bgs", [128, NS])
            prd = sb(P3a, "prd", [128, NS, 3]); yds = sb(P3a, "yds", [128, NS])
            load("sp", sd_tok, sd_tok[:, :], sconvd)
            pb = bank()
            for c in range(4):
                transpose(pb, pb[:, c * 8:(c + 1) * 8], sd_tok[:, c * 128:(c + 1) * 128], ident_f[:8, :8], [sd_tok, ident_f])
            S.op("dve", lambda: DVE.tensor_copy(out=uds[:, :, :, 0:2], in_=pb[:, 0:32].rearrange("p (c b j) -> p c b j", c=4, b=NS)),
                 reads=[pb], writes=[uds])
            for c in range(4):
                ph = bank(); pc = bank(); pg = bank()
                mm(ph, ph[:, :NS], [(w_odd[:, k, 1544 + c * 128:1544 + (c + 1) * 128], xs1T[:, k, :]) for k in range(8)], [w_odd, xs1T])
                mm(pc, pc[:, :NS], [(w_odd[:, k, 2568 + c * 128:2568 + (c + 1) * 128], xs1T[:, k, :]) for k in range(8)], [w_odd, xs1T])
                mm(pg, pg[:, :NS], [(w_odd[:, k, 2056 + c * 128:2056 + (c + 1) * 128], xs1T[:, k, :]) for k in range(8)], [w_odd, xs1T])
                S.op("act", lambda ph=ph: ACT.copy(out=hs[:, :], in_=ph[:, :NS]), reads=[ph], writes=[hs])
                S.op("act", lambda pg=pg: ACT.copy(out=bgs[:, :], in_=pg[:, :NS]), reads=[pg], writes=[bgs])
                S.op("dve", lambda pc=pc, c=c: DVE.tensor_tensor(out=uds[:, c, :, 2], in0=pc[:, :NS], in1=hs[:, :], op=ALU.mult), reads=[pc, hs], writes=[uds])
                S.op("dve", lambda c=c: DVE.tensor_tensor(out=prd[:, :, :], in0=uds[:, c, :, :], in1=cdw[:, c:c + 1, :].to_broadcast([128, NS, 3]), op=ALU.mult),
                     reads=[uds, cdw], writes=[prd])
                S.op("dve", lambda: DVE.tensor_reduce(out=yds[:, :], in_=prd[:, :, :], axis=AX.X, op=ALU.add), reads=[prd], writes=[yds])
                S.op("dve", lambda c=c: DVE.tensor_tensor(out=yTds[:, c, :], in0=yds[:, :], in1=bgs[:, :], op=ALU.mult), reads=[yds, bgs], writes=[yTds])
            for b_ in range(NS):
                pb = bank()
                for c in range(4):
                    transpose(pb, pb[:2, c * 128:(c + 1) * 128], uds[:, c, b_, 1:3], ident_f[:], [uds, ident_f])
                S.op("dve", lambda pb=pb: DVE.tensor_copy(out=tl2[:, :], in_=pb[:2, :]), reads=[pb], writes=[tl2])
                store("sp", tl2, convd_s[b_, :, :], tl2[:, :])
        S.barrier()

        with ExitStack() as P4:
            w_oo = sb(P4, "w_oo", [128, 8, D], BF16)
            load_w_bf(w_oo, w_oo[:], w_out_odd.rearrange("(c p) n -> p c n", p=128))
            gB3 = sb(P4, "gB3", [128, D]); bB3 = sb(P4, "bB3", [128, D])
            load("sp", gB3, gB3[:], ln_mix_odd_g.partition_broadcast(128)); load("sp", bB3, bB3[:], ln_mix_odd_b.partition_broadcast(128))
            otok = sb(P4, "otok", [128, 512]); rden = sb(P4, "rden", [128, 8]); ob = sb(P4, "ob", [128, D], BF16)
            oT = sb(P4, "oT", [128, 8, 128], BF16); x1r = sb(P4, "x1r", [128, D]); r4 = sb(P4, "r4", [128, D]); xo4 = sb(P4, "xo4", [128, D])
            pacc = [pbank[5], pbank[6]]
            srot = [pbank[i] for i in range(4)]
            pa_s = pbank[4]
            si = [0]

            def outproj_ln(n, lhs_chunks, reads_, xres, dst):
                p1 = srot[si[0] % 4]; p2 = srot[(si[0] + 1) % 4]; si[0] += 2
                for hf, pb in ((0, p1), (1, p2)):
                    mm(pb, pb[:n, :], [(lhs_chunks[k], w_oo[:, k, hf * 512:(hf + 1) * 512]) for k in range(8)], reads_ + [w_oo])
                    S.op("dve", lambda hf=hf, pb=pb: DVE.scalar_tensor_tensor(out=r4[:n, hf * 512:(hf + 1) * 512], in0=xres[:n, hf * 512:(hf + 1) * 512],
                                                                              scalar=ALPHA, in1=pb[:n, :], op0=ALU.mult, op1=ALU.add),
                         reads=[xres, pb], writes=[r4])
                layernorm_rows(lnst, r4, n, gB3, bB3, xo4, "p4")
                store("sp", xo4, dst, xo4[:n, :])

            pts = sb(P4, "pts", [128, NS * NPAGES], I32); idx = sb(P4, "idx", [128, NS * NPAGES], I32); iot = sb(P4, "iot", [128, 1], I32)
            S.dma("pool", lambda: POOL.dma_start(out=pts[:], in_=ptab.rearrange("b p -> (b p)").partition_broadcast(128)), writes=[pts])
            S.op("pool", lambda: POOL.iota(iot[:], [[0, 1]], base=0, channel_multiplier=1), writes=[iot])
            S.op("pool", lambda: POOL.tensor_scalar(out=idx[:], in0=pts[:], scalar1=128, scalar2=None, op0=ALU.mult), reads=[pts], writes=[idx])
            S.op("pool", lambda: POOL.tensor_tensor(out=idx[:], in0=idx[:], in1=iot[:].to_broadcast([128, NS * NPAGES]), op=ALU.add),
                 reads=[idx, iot], writes=[idx])
            NP1 = NPAGES + 1
            selb = sb(P4, "selb", [NS, NS, 128]); qb = sb(P4, "qb", [128, 512])
            kpgs = [sb(P4, "kpg%d" % q_, [128, 512]) for q_ in range(4)]; vpgs = [sb(P4, "vpg%d" % q_, [128, 512]) for q_ in range(4)]
            prod2s = [sb(P4, "prod2_%d" % q_, [128, 512]) for q_ in range(2)]; knl = sb(P4, "knl", [128, 512]); vnl = sb(P4, "vnl", [128, 512])
            Ssc = sb(P4, "Ssc", [128, NP1, 8]); lfp = sb(P4, "lfp", [128, NP1, 8]); Gs = sb(P4, "Gs", [128, NP1, 8]); Gt = sb(P4, "Gt", [128, NP1, 8])
            Ee = sb(P4, "Ee", [128, NP1, 8]); esum = sb(P4, "esum", [128, 8]); rd8 = sb(P4, "rd8", [8, 1]); on8 = sb(P4, "on8", [8, 512])
            bdm = sb(P4, "bdm", [8, 512]); oTs = sb(P4, "oTs", [128, 4, NS], BF16); sut = sb(P4, "sut", [128, 128])
            S.op("pool", lambda: POOL.memset(selb[:], 1.0), writes=[selb])
            S.op("pool", lambda: POOL.affine_select(out=selb[:], in_=selb[:], pattern=[[1, NS], [0, 128]], compare_op=ALU.is_equal, fill=0.0,
                                                     base=0, channel_multiplier=-1), reads=[selb], writes=[selb])
            S.op("pool", lambda: POOL.memset(bdm[:], 1.0), writes=[bdm])
            S.op("pool", lambda: POOL.affine_select(out=bdm[:], in_=bdm[:], pattern=[[1, 8], [0, 64]], compare_op=ALU.is_equal, fill=0.0,
                                                     base=0, channel_multiplier=-1), reads=[bdm], writes=[bdm])
            S.op("pool", lambda: POOL.affine_select(out=sut[:], in_=ones_f[:], pattern=[[-1, 128]], compare_op=ALU.is_gt, fill=0.0,
                                                     base=0, channel_multiplier=1), reads=[ones_f], writes=[sut])
            lfpg = [Buf(lfp.t) for _ in range(NPAGES)]

            def sample_gen():
                for b_ in range(NS):
                    pb = srot[si[0] % 4]; si[0] += 1
                    mm(pb, pb[:, :], [(selb[:, b_, :], qs_tok[:, :])], [selb, qs_tok])
                    S.op("act", lambda pb=pb: ACT.copy(out=qb[:, :], in_=pb[:, :]), reads=[pb], writes=[qb])
                    S.op("pool", lambda: POOL.memset(lfp[:, NPAGES, :], 0.0), writes=[lfp])
                    for p in range(NPAGES):
                        S.dma("pool", lambda p=p: POOL.indirect_dma_start(out=lfp[:, p, :], out_offset=None, in_=cache_lf,
                                                                           in_offset=bass.IndirectOffsetOnAxis(ap=idx[:, b_ * NPAGES + p:b_ * NPAGES + p + 1], axis=0)),
                              reads=[idx], writes=[lfpg[p]])
                        yield
                    S.dma("sp", lambda: SP.dma_start(out=lfp[0:1, NPAGES, :], in_=lfnew[b_:b_ + 1, :]), reads=[lfnew], writes=[lfp])
                    NPG = NPAGES
                    p1 = srot[si[0] % 4]; p2 = srot[(si[0] + 1) % 4]; p3_ = srot[(si[0] + 2) % 4]; si[0] += 3
                    lf2 = lfp[:, 0:NPG, :].rearrange("p a h -> p (a h)")
                    mm(p1, p1[:, 0:NPG * 8], [(sut[:], lf2)], [sut, lfp] + lfpg)
                    mm(p2, p2[:, 0:NPG * 8], [(ones_f[:], lf2)], [ones_f, lfp] + lfpg)
                    mm(p3_, p3_[:, 0:8], [(selb[:, b_, :], lfnew[:, :])], [selb, lfnew])
                    S.op("dve", lambda p2=p2: DVE.tensor_copy(out=Gt[:, 0:NPG, :], in_=p2[:, 0:NPG * 8].rearrange("p (a h) -> p a h", h=8)), reads=[p2], writes=[Gt])
                    S.op("dve", lambda: DVE.tensor_copy(out=Ee[:, 0:NPG, :], in_=Gt[:, 0:NPG, :]), reads=[Gt], writes=[Ee])
                    src, dst = Gt, Gs
                    sft = 1
                    while sft < NPG:
                        S.op("dve", lambda src=src, dst=dst, sft=sft: DVE.tensor_tensor(out=dst[:, 0:NPG - sft, :], in0=src[:, 0:NPG - sft, :],
                                                                                     in1=src[:, sft:NPG, :], op=ALU.add), reads=[src], writes=[dst])
                        S.op("dve", lambda src=src, dst=dst, sft=sft: DVE.tensor_copy(out=dst[:, NPG - sft:NPG, :], in_=src[:, NPG - sft:NPG, :]),
                             reads=[src], writes=[dst])
                        src, dst = dst, src
                        sft *= 2
                    incl = src
                    other = dst
                    S.op("dve", lambda: DVE.tensor_tensor(out=other[:, 0:NPG, :], in0=incl[:, 0:NPG, :], in1=Ee[:, 0:NPG, :], op=ALU.subtract), reads=[incl, Ee], writes=[other])
                    S.op("dve", lambda p1=p1: DVE.tensor_tensor(out=other[:, 0:NPG, :], in0=other[:, 0:NPG, :], in1=p1[:, 0:NPG * 8].rearrange("p (a h) -> p a h", h=8),
                                                                op=ALU.add), reads=[other, p1], writes=[other])
                    S.op("dve", lambda p3_=p3_: DVE.tensor_copy(out=esum[:, :], in_=p3_[:, 0:8]), reads=[p3_], writes=[esum])
                    S.op("dve", lambda: DVE.tensor_tensor(out=other[:, 0:NPG, :], in0=other[:, 0:NPG, :], in1=esum[:, :].unsqueeze(1).to_broadcast([128, NPG, 8]),
                                                          op=ALU.add), reads=[other, esum], writes=[other])
                    Gfin = other
                    S.op("pool", lambda: POOL.memset(Gfin[:, NPAGES, :], 0.0), writes=[Gfin])
                    S.op("pool", lambda: POOL.affine_select(
                        out=Gfin[:, NPAGES, :], in_=Gfin[:, NPAGES, :], pattern=[[0, 8]], compare_op=ALU.is_equal, fill=NEG, base=0, channel_multiplier=1),
                        reads=[Gfin], writes=[Gfin])
                    S.op("pool", lambda: POOL.memset(knl[:, :], 0.0), writes=[knl])
                    S.dma("sp", lambda: SP.dma_start(out=knl[0:1, :], in_=knew[b_:b_ + 1, :]), reads=[knew], writes=[knl])
                    for p in range(NP1):
                        if p < NPAGES:
                            kpg = kpgs[p % 4]
                            S.dma("pool", lambda p=p, kpg=kpg: POOL.indirect_dma_start(out=kpg[:, :], out_offset=None, in_=cache_k,
                                                                               in_offset=bass.IndirectOffsetOnAxis(ap=idx[:, b_ * NPAGES + p:b_ * NPAGES + p + 1], axis=0)),
                                  reads=[idx], writes=[kpg])
                        else:
                            kpg = knl
                        if p >= 2:
                            pq = p - 2
                            kq = kpgs[pq % 4]; prod2 = prod2s[pq % 2]
                            S.op("dve", lambda kq=kq, prod2=prod2: DVE.tensor_tensor(out=prod2[:, :], in0=kq[:, :], in1=qb[:, :], op=ALU.mult), reads=[kq, qb], writes=[prod2])
                            S.op("dve", lambda pq=pq, prod2=prod2: DVE.tensor_reduce(out=Ssc[:, pq, :], in_=prod2[:, :].rearrange("p (h d) -> p h d", h=8), axis=AX.X, op=ALU.add),
                                 reads=[prod2], writes=[Ssc])
                        yield
                    for pq in (NP1 - 2, NP1 - 1):
                        kq = kpgs[pq % 4] if pq < NPAGES else knl
                        prod2 = prod2s[pq % 2]
                        S.op("dve", lambda kq=kq, prod2=prod2: DVE.tensor_tensor(out=prod2[:, :], in0=kq[:, :], in1=qb[:, :], op=ALU.mult), reads=[kq, qb], writes=[prod2])
                        S.op("dve", lambda pq=pq, prod2=prod2: DVE.tensor_reduce(out=Ssc[:, pq, :], in_=prod2[:, :].rearrange("p (h d) -> p h d", h=8), axis=AX.X, op=ALU.add),
                             reads=[prod2], writes=[Ssc])
                    S.op("dve", lambda: DVE.tensor_tensor(out=Ssc[:, :, :], in0=Ssc[:, :, :], in1=Gfin[:, :, :], op=ALU.add), reads=[Ssc, Gfin], writes=[Ssc])
                    S.op("act", lambda: ACT.activation(out=Ee[:, :, :], in_=Ssc[:, :, :], func=AF.Exp), reads=[Ssc], writes=[Ee])
                    S.op("dve", lambda: DVE.tensor_reduce(out=esum[:, :], in_=Ee[:, :, :].rearrange("p a h -> p h a"), axis=AX.X, op=ALU.add), reads=[Ee], writes=[esum])
                    pa = pa_s
                    S.op("pool", lambda: POOL.memset(vnl[:, :], 0.0), writes=[vnl])
                    S.dma("sp", lambda: SP.dma_start(out=vnl[0:1, :], in_=vnew[b_:b_ + 1, :]), reads=[vnew], writes=[vnl])
                    for p in range(NP1 + 2):
                        if p < NPAGES:
                            vpg = vpgs[p % 4]
                            S.dma("pool", lambda p=p, vpg=vpg: POOL.indirect_dma_start(out=vpg[:, :], out_offset=None, in_=cache_v,
                                                                               in_offset=bass.IndirectOffsetOnAxis(ap=idx[:, b_ * NPAGES + p:b_ * NPAGES + p + 1], axis=0)),
                                  reads=[idx], writes=[vpg])
                        if p >= 2:
                            pq = p - 2
                            vq = vpgs[pq % 4] if pq < NPAGES else vnl
                            S.op("pe", lambda pq=pq, pa=pa, vq=vq: PE.matmul(pa[:8, :], lhsT=Ee[:, pq, :], rhs=vq[:, :], start=(pq == 0), stop=(pq == NP1 - 1)),
                                 reads=[Ee, vq], writes=[pa])
                        yield
                    pd = srot[si[0] % 4]; si[0] += 1
                    mm(pd, pd[:8, 0:1], [(esum[:, :], ones_f[:, 0:1])], [esum, ones_f])
                    S.op("dve", lambda pd=pd: DVE.reciprocal(out=rd8[:, :], in_=pd[:8, 0:1]), reads=[pd], writes=[rd8])
                    S.op("dve", lambda pa=pa: DVE.scalar_tensor_tensor(out=on8[:, :], in0=pa[:8, :], scalar=rd8[:, 0:1], in1=bdm[:, :], op0=ALU.mult, op1=ALU.mult),
                         reads=[pa, rd8, bdm], writes=[on8])
                    pb = srot[si[0] % 4]; si[0] += 1
                    for c in range(4):
                        mm(pb, pb[:, c:c + 1], [(on8[:, c * 128:(c + 1) * 128], ones_f[:8, 0:1])], [on8, ones_f])
                    S.op("act", lambda pb=pb: ACT.copy(out=oTs[:, :, b_], in_=pb[:, 0:4]), reads=[pb], writes=[oTs])
            PTs = [sb(P4, "PT%d" % q_, [128, 128], BF16) for q_ in range(4)]
            biases = [sb(P4, "bias%d" % q_, [128, 8]) for q_ in range(4)]
            pend = []
            cnt_ = [0]
            sg_ = sample_gen()
            sdone = [False]

            def step_sample(n):
                for _ in range(n):
                    if sdone[0]:
                        return
                    try:
                        next(sg_)
                    except StopIteration:
                        sdone[0] = True

            def flush(keep):
                while len(pend) > keep:
                    (pa_, col_, PT_, j_, h_, st_, sp_) = pend.pop(0)
                    S.op("pe", lambda: PE.matmul(pa_[:, col_:col_ + 65], lhsT=PT_[:, :], rhs=Vx[:, j_, h_, :], start=st_, stop=sp_), reads=[PT_, Vx], writes=[pa_])

            for i in range(16):
                load("sp", x1r, x1r[:, :], x1_scr[HALF + i * 128:HALF + (i + 1) * 128, :])
                for j in range(17 + i):
                    bias_ij = biases[(cnt_[0] // 8) % 4]
                    S.op("dve", lambda i=i, j=j, bias_ij=bias_ij: DVE.tensor_tensor(out=bias_ij[:, :], in0=negFm[:, j, :], in1=Fend[:, i, :], op=ALU.add),
                         reads=[negFm, Fend], writes=[bias_ij])
                    for h in range(8):
                        c, pbase = h // 2, (h % 2) * 64
                        pb = srot[si[0] % 4]; si[0] += 1
                        PT_ = PTs[cnt_[0] % 4]; cnt_[0] += 1
                        pairs = [(KT[pbase:pbase + 64, c, j * 128:(j + 1) * 128], QT[pbase:pbase + 64, c, i * 128:(i + 1) * 128])]
                        if j == 16 + i:
                            pairs.append((ident_b[:], maskT[:]))
                        mm(pb, pb[:, 0:128], pairs, [KT, QT, ident_b, maskT])
                        S.op("act", lambda pb=pb, h=h, PT_=PT_, bias_ij=bias_ij: ACT.activation(out=PT_[:, :], in_=pb[:, 0:128], func=AF.Exp, bias=bias_ij[:, h:h + 1], scale=1.0),
                             reads=[pb, bias_ij], writes=[PT_])
                        pend.append((pacc[h // 4], (h % 4) * 65, PT_, j, h, (j == 0), (j == 16 + i)))
                        flush(2)
                    step_sample(2)
                flush(0)
                for hh in range(2):
                    pa = pacc[hh]
                    S.op("dve", lambda pa=pa, hh=hh: DVE.reciprocal(out=rden[:, hh * 4:(hh + 1) * 4], in_=pa[:, 0:260].rearrange("p (h d) -> p h d", h=4)[:, :, 64]),
                         reads=[pa], writes=[rden])
                    S.op("dve", lambda pa=pa, hh=hh: DVE.tensor_tensor(
                        out=otok[:, hh * 256:(hh + 1) * 256].rearrange("p (h d) -> p h d", h=4),
                        in0=pa[:, 0:260].rearrange("p (h d) -> p h d", h=4)[:, :, 0:64],
                        in1=rden[:, hh * 4:(hh + 1) * 4].unsqueeze(2).to_broadcast([128, 4, 64]), op=ALU.mult), reads=[pa, rden], writes=[otok])
                S.op("pool", lambda: POOL.tensor_copy(out=ob[:, 0:512], in_=otok[:, :]), reads=[otok], writes=[ob])
                for k in range(4):
                    transpose(ptr, ptr[:, k * 128:(k + 1) * 128], ob[:, k * 128:(k + 1) * 128], ident_b[:], [ob, ident_b])
                S.op("act", lambda: ACT.copy(out=oT[:, 0:4, :], in_=ptr[:, 0:512].rearrange("p (k t) -> p k t", k=4)), reads=[ptr], writes=[oT])
                outproj_ln(128, [oT[:, k, :] for k in range(4)] + [yTd[:, c, i * 128:(i + 1) * 128] for c in range(4)], [oT, yTd], x1r,
                           x2_scr[i * 128:(i + 1) * 128, :])

            step_sample(100000)
            outproj_ln(NS, [oTs[:, k, :] for k in range(4)] + [yTds[:, c, :] for c in range(4)], [oTs, yTds], xs1, x2_scr[HALF:HALF + NS, :])
        S.barrier()
    S.barrier()

    moe = [(moe_w1[e], moe_w3[e], moe_w2[e], DFE, e) for e in range(NE)]
    gated_pass("mo", rows(x2_scr, 0, HALF) + [(x2_scr[HALF:HALF + NS, :], NS)], moe, ln_ffn_odd_g, ln_ffn_odd_b,
               rows(y_own, 0, HALF) + [(y_s[:, :], NS)], router=router_w)
    S.barrier()
    return


POOL_WINDOWS = (2, 4, 8, 16)
W_NAMES = ["w_in_even", "pool_w", "pool_scale", "conv_b_w", "conv_b_bias", "conv_ln_g", "conv_ln_b", "w_out_even",
           "ln_mix_even_g", "ln_mix_even_b", "ffn_w1", "ffn_w3", "ffn_w2", "ln_ffn_even_g", "ln_ffn_even_b",
           "w_in_odd", "forget_bias", "conv_d_w", "w_out_odd", "ln_mix_odd_g", "ln_mix_odd_b", "router_w",
           "moe_w1", "moe_w3", "moe_w2", "ln_ffn_odd_g", "ln_ffn_odd_b"]


def make_core_inputs(inp, c, compact_cache=False):
    b, h = c // 2, c % 2
    f = np.float32
    xp = np.asarray(inp["x_prompt"])
    m = {}
    if h == 1:
        m["xloc"] = np.ascontiguousarray(xp[b])
    else:
        m["xloc"] = np.concatenate([np.zeros((HALF, D), f), xp[b, :HALF]], axis=0)
    sl = slice(NS * c, NS * c + NS)
    m["xs"] = np.ascontiguousarray(np.asarray(inp["x_sample"])[sl, 0])
    m["spool"] = np.ascontiguousarray(np.asarray(inp["state_pool"])[0, sl]).reshape(NS * 15, 512)
    m["sconvb"] = np.ascontiguousarray(np.asarray(inp["state_conv_b"])[0, sl]).reshape(NS * 30, 512)
    m["sconvd"] = np.ascontiguousarray(np.asarray(inp["state_conv_d"])[0, sl]).reshape(NS * 2, 512)
    pt = np.asarray(inp["page_table"])[sl].astype(np.int32)
    ck, cv, cl = (np.asarray(inp[k])[0] for k in ("cache_k", "cache_v", "cache_logf"))
    if compact_cache:
        ids = pt.reshape(-1)
        ck, cv, cl = ck[ids], cv[ids], cl[ids]
        pt = np.arange(ids.size, dtype=np.int32).reshape(pt.shape)
    m["cache_k"] = ck.reshape(-1, 512); m["cache_v"] = cv.reshape(-1, 512); m["cache_lf"] = cl.reshape(-1, 8)
    m["ptab"] = np.ascontiguousarray(pt)
    pm = np.zeros((128, 32), f)
    if h == 0:
        pm[:, :16] = NEG
    m["pmask"] = pm
    gpos = np.arange(SEQ) if h == 1 else np.concatenate([np.full(HALF, 100000), np.arange(HALF)])
    m["invcnt"] = np.stack([1.0 / np.minimum(gpos + 1, w) for w in POOL_WINDOWS]).astype(f)
    m["hflag"] = np.full((128, 1), float(h), f)
    for k in W_NAMES:
        a = np.asarray(inp[k])
        m[k] = np.ascontiguousarray(a[0]) if a.shape[0] == 1 else a
    return m


def assemble(results, ncores=8):
    f = np.float32
    nb = ncores // 2
    y = np.zeros((nb, SEQ, D), f); ys = np.zeros((ncores * NS, 1, D), f)
    pool_p = np.zeros((1, nb, 15, 512), f); pool_s = np.zeros((1, ncores * NS, 15, 512), f)
    convb_p = np.zeros((1, nb, 30, 512), f); convb_s = np.zeros((1, ncores * NS, 30, 512), f)
    k_p = np.zeros((1, nb, SEQ, 8, 64), f); k_s = np.zeros((1, ncores * NS, 1, 8, 64), f)
    v_p = np.zeros((1, nb, SEQ, 8, 64), f); v_s = np.zeros((1, ncores * NS, 1, 8, 64), f)
    lf_p = np.zeros((1, nb, SEQ, 8), f); lf_s = np.zeros((1, ncores * NS, 1, 8), f)
    cd_p = np.zeros((1, nb, 2, 512), f); cd_s = np.zeros((1, ncores * NS, 2, 512), f)
    for c, r in enumerate(results):
        b, h = c // 2, c % 2
        ts = slice(h * HALF, (h + 1) * HALF); ss = slice(c * NS, (c + 1) * NS)
        y[b, ts] = r["y_own"]; ys[ss, 0] = r["y_s"]
        k_p[0, b, ts] = np.asarray(r["k_own"]).reshape(HALF, 8, 64); v_p[0, b, ts] = np.asarray(r["v_own"]).reshape(HALF, 8, 64)
        lf_p[0, b, ts] = r["lf_own"]
        k_s[0, ss, 0] = np.asarray(r["k_s"]).reshape(NS, 8, 64); v_s[0, ss, 0] = np.asarray(r["v_s"]).reshape(NS, 8, 64)
        lf_s[0, ss, 0] = r["lf_s"]
        pool_s[0, ss] = r["pool_s"]; convb_s[0, ss] = r["convb_s"]; cd_s[0, ss] = r["convd_s"]
        if h == 1:
            pool_p[0, b] = r["pool_tail"]; convb_p[0, b] = r["convb_tail"]; cd_p[0, b] = r["convd_tail"]
    return (y, ys, pool_p, pool_s, convb_p, convb_s, k_p, k_s, v_p, v_s, lf_p, lf_s, cd_p, cd_s)


def kernel(**inputs):
    nc = build(n_pool=2560, dev=False)
    maps = [make_core_inputs(inputs, c) for c in range(8)]
    res = run_bass_kernel_spmd(nc, maps, core_ids=list(range(8)))
    return assemble(res.results, 8)
```
